# Optimizing a Trainium2 kernel written in Bass

```python
import math
import jax, jax.numpy as jnp
from jax import lax
import numpy as np

D_MODEL = 1024
BATCH = 16
SEQ = 4096
DEPTH = 2

CHUNK = 64
QBLK = 128
MEM_LEN = 256
BRANCH_WIDTH = D_MODEL // 2
SB_HEAD_DIM = 64
SB_HEADS = BRANCH_WIDTH // SB_HEAD_DIM
DIFF_HEAD_DIM = 64
DIFF_HEADS = BRANCH_WIDTH // (2 * DIFF_HEAD_DIM)
MEM_HEAD_DIM = 128
MEM_HEADS = BRANCH_WIDTH // MEM_HEAD_DIM
N_BRANCHES = 3
IN_WIDTH = 7 * BRANCH_WIDTH + N_BRANCHES * D_MODEL
NUM_BUCKETS = 32
MAX_DISTANCE = 128
N_EXPERTS = 32
TOP_K = 4
D_FF = D_MODEL
SWIGLU_LIMIT = 7.0
SWIGLU_ALPHA = 1.702
LN_EPS = 1e-5
RMS_EPS = 1e-5
DEEPNORM_ALPHA = (2 * DEPTH) ** 0.25
DEEPNORM_BETA = (8 * DEPTH) ** -0.25

kernel_name = "hybrid_stickbreak_diffattn_memxattn_moe_deepnorm"

F32 = jnp.float32


def _layer_norm(x, g, b):
    xf = x.astype(F32)
    mu = jnp.mean(xf, axis=-1, keepdims=True)
    xc = xf - mu
    var = jnp.mean(xc * xc, axis=-1, keepdims=True)
    return (xc * lax.rsqrt(var + LN_EPS) * g.astype(F32) + b.astype(F32)).astype(x.dtype)


def _t5_bucket(rel):
    half = NUM_BUCKETS // 2
    max_exact = half // 2
    n = jnp.abs(rel)
    nf = jnp.maximum(n, 1).astype(F32)
    large = max_exact + (jnp.log(nf / max_exact) / math.log(MAX_DISTANCE / max_exact)
                         * (half - max_exact)).astype(jnp.int32)
    large = jnp.minimum(large, half - 1)
    return jnp.where(rel > 0, half, 0) + jnp.where(n < max_exact, n, large)


def _query_blocks(q):
    B, S, H, d = q.shape
    return q.astype(F32).reshape(B, S // QBLK, QBLK, H, d).transpose(1, 0, 3, 2, 4)


def _unblock(o):
    nb, B, Q, H, e = o.shape
    return o.transpose(1, 0, 2, 3, 4).reshape(B, nb * Q, H, e)


def _stick_breaking_attention(q, k, v):
    B, S = q.shape[:2]
    nb = S // QBLK
    kf, vf = k.astype(F32), v.astype(F32)
    kpos = jnp.arange(S)
    scale = SB_HEAD_DIM ** -0.5

    def block(args):
        qblk, i = args
        t = i * QBLK + jnp.arange(QBLK)
        z = jnp.einsum('bhqd,bshd->bhqs', qblk, kf) * scale
        earlier = kpos[None, :] < t[:, None]
        log_1mb = jnp.where(earlier, jax.nn.log_sigmoid(-z), 0.0)
        between = lax.cumsum(log_1mb, axis=3, reverse=True) - log_1mb
        a = jnp.where(earlier, jnp.exp(jax.nn.log_sigmoid(z) + between), 0.0)
        return jnp.einsum('bhqs,bshd->bqhd', a, vf)

    o = _unblock(lax.map(block, (_query_blocks(q), jnp.arange(nb))))
    return o.reshape(B, S, BRANCH_WIDTH).astype(q.dtype)


def _diff_attention(q, k, v, lam, lam_init, subln_g, rel_bias):
    B, S = q.shape[:2]
    nb = S // QBLK
    kf, vf = k.astype(F32), v.astype(F32)
    kpos = jnp.arange(S)
    table = rel_bias.astype(F32)
    scale = DIFF_HEAD_DIM ** -0.5

    def block(args):
        qblk, i = args
        t = i * QBLK + jnp.arange(QBLK)
        bias = table[_t5_bucket(kpos[None, :] - t[:, None])].transpose(2, 0, 1)
        logits = jnp.einsum('bhqd,bshd->bhqs', qblk, kf) * scale + bias
        visible = (kpos[None, :] // CHUNK) <= (t[:, None] // CHUNK)
        p = jax.nn.softmax(jnp.where(visible, logits, -jnp.inf), axis=-1)
        p = p.reshape(B, DIFF_HEADS, 2, QBLK, S)
        a = p[:, :, 0] - lam * p[:, :, 1]
        return jnp.einsum('bhqs,bshe->bqhe', a, vf)

    o = _unblock(lax.map(block, (_query_blocks(q), jnp.arange(nb))))
    o = o * lax.rsqrt(jnp.mean(o * o, axis=-1, keepdims=True) + RMS_EPS) * subln_g.astype(F32)
    return (o * (1.0 - lam_init)).reshape(B, S, BRANCH_WIDTH).astype(q.dtype)


def _memory_attention(q, mem_k, mem_v):
    B, S = q.shape[:2]
    logits = jnp.einsum('bshd,bmhd->bhsm', q.astype(F32), mem_k.astype(F32)) * MEM_HEAD_DIM ** -0.5
    p = jax.nn.softmax(logits, axis=-1)
    o = jnp.einsum('bhsm,bmhd->bshd', p, mem_v.astype(F32))
    return o.reshape(B, S, BRANCH_WIDTH).astype(q.dtype)


def _mixer(x, mem, w_in, b_gate, diff_lambda, diff_subln_g, rel_bias, w_mem_kv, w_branch, w_out, layer_idx):
    B, S, _ = x.shape
    W = BRANCH_WIDTH
    proj = x @ w_in
    sb_q, sb_k, sb_v, df_q, df_k, df_v, mem_q = [proj[..., i * W:(i + 1) * W] for i in range(7)]
    gates = jax.nn.sigmoid((proj[..., 7 * W:] + b_gate).astype(F32)).astype(x.dtype)
    gates = gates.reshape(B, S, N_BRANCHES, D_MODEL)

    hd = (B, S, SB_HEADS, SB_HEAD_DIM)
    y_sb = _stick_breaking_attention(sb_q.reshape(hd), sb_k.reshape(hd), sb_v.reshape(hd))

    lam_init = 0.8 - 0.6 * math.exp(-0.3 * layer_idx)
    lf = diff_lambda.astype(F32)
    lam = jnp.exp(jnp.sum(lf[0] * lf[1])) - jnp.exp(jnp.sum(lf[2] * lf[3])) + lam_init
    qk_shape = (B, S, 2 * DIFF_HEADS, DIFF_HEAD_DIM)
    y_df = _diff_attention(df_q.reshape(qk_shape), df_k.reshape(qk_shape),
                           df_v.reshape(B, S, DIFF_HEADS, 2 * DIFF_HEAD_DIM),
                           lam, lam_init, diff_subln_g, rel_bias)

    M = mem.shape[1]
    mem_kv = mem @ w_mem_kv
    mem_k = mem_kv[..., :W].reshape(B, M, MEM_HEADS, MEM_HEAD_DIM)
    mem_v = mem_kv[..., W:].reshape(B, M, MEM_HEADS, MEM_HEAD_DIM)
    y_mem = _memory_attention(mem_q.reshape(B, S, MEM_HEADS, MEM_HEAD_DIM), mem_k, mem_v)

    merged = (gates[:, :, 0] * (y_sb @ w_branch[0])
              + gates[:, :, 1] * (y_df @ w_branch[1])
              + gates[:, :, 2] * (y_mem @ w_branch[2]))
    return merged @ w_out


def _moe(x, router_w, router_b, w_gate_up, b_gate_up, w_down, b_down):
    B, S, D = x.shape
    xt = x.reshape(B * S, D)
    logits = (xt @ router_w).astype(F32) + router_b.astype(F32)
    top_val, top_idx = lax.top_k(logits, TOP_K)
    top_w = jax.nn.softmax(top_val, axis=-1)
    combine = jnp.einsum('tk,tke->et', top_w,
                         jax.nn.one_hot(top_idx, N_EXPERTS, dtype=F32)).astype(x.dtype)

    def expert(acc, p):
        wgu, bgu, wd, bd, c = p
        h = xt @ wgu + bgu
        gate = jnp.minimum(h[:, :D_FF], SWIGLU_LIMIT)
        up = jnp.clip(h[:, D_FF:], -SWIGLU_LIMIT, SWIGLU_LIMIT)
        y = ((up + 1.0) * (gate * jax.nn.sigmoid(SWIGLU_ALPHA * gate))) @ wd + bd
        return acc + c[:, None] * y, None

    out, _ = lax.scan(expert, jnp.zeros_like(xt), (w_gate_up, b_gate_up, w_down, b_down, combine))
    return out.reshape(B, S, D)


def setup_inputs(seed: int = 0) -> dict:
    key = jax.random.key(seed)
    ks = jax.random.split(key, 20)
    L = DEPTH
    W = BRANCH_WIDTH

    def nrm(k, shape, scale):
        return jax.random.normal(k, shape, F32) * scale

    return {
        "x": nrm(ks[0], (BATCH, SEQ, D_MODEL), 1.0),
        "mem": nrm(ks[1], (BATCH, MEM_LEN, D_MODEL), 1.0),
        "w_in": nrm(ks[2], (L, D_MODEL, IN_WIDTH), D_MODEL ** -0.5),
        "b_gate": nrm(ks[3], (L, N_BRANCHES * D_MODEL), 0.1),
        "diff_lambda": nrm(ks[4], (L, 4, DIFF_HEAD_DIM), 0.1),
        "diff_subln_g": 1.0 + nrm(ks[5], (L, 2 * DIFF_HEAD_DIM), 0.02),
        "rel_bias": nrm(ks[6], (NUM_BUCKETS, 2 * DIFF_HEADS), 0.5),
        "w_mem_kv": nrm(ks[7], (L, D_MODEL, 2 * W), D_MODEL ** -0.5),
        "w_branch": nrm(ks[8], (L, N_BRANCHES, W, D_MODEL), W ** -0.5),
        "w_out": nrm(ks[9], (L, D_MODEL, D_MODEL), D_MODEL ** -0.5 * DEEPNORM_BETA),
        "ln1_g": 1.0 + nrm(ks[10], (L, D_MODEL), 0.02),
        "ln1_b": nrm(ks[11], (L, D_MODEL), 0.02),
        "router_w": nrm(ks[12], (L, D_MODEL, N_EXPERTS), D_MODEL ** -0.5),
        "router_b": nrm(ks[13], (L, N_EXPERTS), 0.01),
        "w_gate_up": nrm(ks[14], (L, N_EXPERTS, D_MODEL, 2 * D_FF), D_MODEL ** -0.5),
        "b_gate_up": nrm(ks[15], (L, N_EXPERTS, 2 * D_FF), 0.02),
        "w_down": nrm(ks[16], (L, N_EXPERTS, D_FF, D_MODEL), D_FF ** -0.5 * DEEPNORM_BETA),
        "b_down": nrm(ks[17], (L, N_EXPERTS, D_MODEL), 0.02),
        "ln2_g": 1.0 + nrm(ks[18], (L, D_MODEL), 0.02),
        "ln2_b": nrm(ks[19], (L, D_MODEL), 0.02),
    }


def reference(x, mem, w_in, b_gate, diff_lambda, diff_subln_g, rel_bias, w_mem_kv, w_branch, w_out,
              ln1_g, ln1_b, router_w, router_b, w_gate_up, b_gate_up, w_down, b_down, ln2_g, ln2_b):
    for l in range(DEPTH):
        h = _mixer(x, mem, w_in[l], b_gate[l], diff_lambda[l], diff_subln_g[l], rel_bias,
                   w_mem_kv[l], w_branch[l], w_out[l], l)
        x = _layer_norm(DEEPNORM_ALPHA * x + h, ln1_g[l], ln1_b[l])
        f = _moe(x, router_w[l], router_b[l], w_gate_up[l], b_gate_up[l], w_down[l], b_down[l])
        x = _layer_norm(DEEPNORM_ALPHA * x + f, ln2_g[l], ln2_b[l])
    return x
```

```python
import numpy as np
import concourse.bass as bass
import concourse.mybir as mybir
from concourse.bass_utils import run_bass_kernel_spmd

F32 = mybir.dt.float32
BF16 = mybir.dt.bfloat16
AF = mybir.ActivationFunctionType
ALU = mybir.AluOpType
AX = mybir.AxisListType

NCORES = 8
D = 1024
SEQ = 4096
NSEQ = 2
T = NSEQ * SEQ
DEPTH = 2
W = 512
INW = 6656
NE = 32
MEM = 256
ALPHA = (2 * DEPTH) ** 0.25
NEG = -30000.0


class Res:
    __slots__ = ("name", "w", "r", "dram", "excl")

    def __init__(self, name="", dram=False, excl=False):
        self.name = name; self.w = {}; self.r = {}; self.dram = dram; self.excl = excl


class Sched:
    ENGS = ("pe", "act", "dve", "pool", "sp")

    def __init__(self, nc):
        self.nc = nc
        self.ops = {e: [] for e in self.ENGS}
        self.sems = {}; self.cnt = {}
        self.seen = {e: {} for e in self.ENGS}
        self.pending = {e: False for e in self.ENGS}
        for e in ("pe", "act", "dve", "pool"):
            self.newsem(e)
        self.sb_off = 0
        self.sb_max = 0
        self.ARENA = 203 * 1024
        self.BASE = 20480
        self.nalloc = 0

    def newsem(self, key):
        self.sems[key] = self.nc.alloc_semaphore(f"s_{key}")
        self.cnt[key] = 0

    def sb(self, shape, dtype, name=None):
        esz = 4 if dtype == F32 else 2
        n = 1
        for s in shape[1:]:
            n *= s
        nbytes = (n * esz + 31) // 32 * 32
        off = self.sb_off
        self.sb_off += nbytes
        self.sb_max = max(self.sb_max, self.sb_off)
        assert self.sb_off <= self.ARENA, f"SBUF overflow {self.sb_off} ({name})"
        self.nalloc += 1
        t = self.nc.alloc_sbuf_tensor_at(f"{name or 't'}_{self.nalloc}", [128] + list(shape[1:]), dtype, offset=self.BASE + off)
        v = t[:]
        if shape[0] != 128:
            v = v[0:shape[0]]
        return v

    def _deps(self, eng, reads, writes):
        need = {}
        for r in reads:
            for k, v in r.w.items():
                if need.get(k, 0) < v: need[k] = v
        for w in writes:
            for k, v in w.w.items():
                if need.get(k, 0) < v: need[k] = v
            for k, v in w.r.items():
                if need.get(k, 0) < v: need[k] = v
        waits = []
        seen = self.seen[eng]
        for k, v in need.items():
            if eng == "pe" and k == "pe":
                continue
            if seen.get(k, 0) >= v:
                continue
            seen[k] = v
            waits.append((k, v))
        return waits

    def _mark(self, k, val, reads, writes):
        for r in reads:
            if r.r.get(k, 0) < val: r.r[k] = val
        for w in writes:
            if w.dram:
                if w.w.get(k, 0) < val: w.w[k] = val
            else:
                w.w = {k: val}; w.r = {}

    def op(self, eng, fn, reads=(), writes=(), inc=True):
        ex = [r for r in reads if r.excl]
        if ex:
            reads = [r for r in reads if not r.excl]
            writes = list(writes) + ex
        waits = self._deps(eng, reads, writes)
        val = self.cnt[eng] + 1
        if inc:
            self.cnt[eng] = val; self.pending[eng] = False
        else:
            self.pending[eng] = True
        self._mark(eng, val, reads, writes)
        self.ops[eng].append((waits, fn, (eng, 1) if inc else None))

    def dma(self, queue, out, in_, reads, writes, semkey, **kw):
        waits = self._deps(queue, reads, writes)
        if semkey not in self.sems:
            self.newsem(semkey)
        self.cnt[semkey] += 16
        val = self.cnt[semkey]
        self._mark(semkey, val, reads, writes)
        self.ops[queue].append((waits, (lambda e: e.dma_start(out=out, in_=in_, **kw)), (semkey, 16)))

    def barrier(self):
        for e in self.ENGS:
            assert not self.pending[e], e
            waits = []
            for k, v in self.cnt.items():
                if v == 0 or (k == e):
                    continue
                if self.seen[e].get(k, 0) >= v:
                    continue
                self.seen[e][k] = v
                waits.append((k, v))
            if waits:
                self.ops[e].append((waits, None, None))

    def new_phase(self):
        self.barrier()
        self.sb_off = self.persist_off

    def emit(self):
        self.barrier()
        nc = self.nc
        sems = self.sems
        ops = self.ops

        def run(name):
            def f(e):
                for waits, fn, inc in ops[name]:
                    for k, v in waits:
                        e.wait_ge(sems[k], v)
                    if fn is None:
                        continue
                    ins = fn(e)
                    if inc is not None:
                        ins.then_inc(sems[inc[0]], inc[1])
            return f
        with nc.Block() as block:
            block.tensor(run("pe"))
            block.scalar(run("act"))
            block.vector(run("dve"))
            block.gpsimd(run("pool"))
            block.sync(run("sp"))
        print("ops:", {k: len(v) for k, v in ops.items()}, "sems:", len(sems), "sbuf:", self.sb_max, flush=True)


def t5_bucket_np(rel):
    half = 16; max_exact = 8
    n = np.abs(rel)
    nf = np.maximum(n, 1).astype(np.float32)
    large = max_exact + (np.log(nf / max_exact) / np.log(128 / max_exact) * (half - max_exact)).astype(np.int32)
    large = np.minimum(large, half - 1)
    return np.where(rel > 0, half, 0) + np.where(n < max_exact, n, large)


def host_consts():
    c = {}
    c["ident"] = np.eye(128, dtype=np.float32)
    c["antiid"] = np.eye(128, dtype=np.float32)[::-1].copy()
    j = np.arange(128)[:, None]; s = np.arange(128)[None, :]
    c["uincl_neg"] = np.where(j >= s, -1.0, 0.0).astype(np.float32)
    c["ones_neg"] = -np.ones((128, 128), np.float32)
    c["ones"] = np.ones((128, 128), np.float32)
    c["sbmask"] = np.where(j >= s, NEG, 0.0).astype(np.float32)
    rel = np.arange(384) - 255
    b = t5_bucket_np(rel)
    oh = np.zeros((32, 384), np.float32)
    oh[b, np.arange(384)] = 1.0
    c["bucket_oh"] = oh
    return c


CONST_SHAPES = {"ident": [128, 128], "antiid": [128, 128], "uincl_neg": [128, 128], "ones_neg": [128, 128],
                "ones": [128, 128], "sbmask": [128, 128], "bucket_oh": [32, 384]}


class Builder:
    def __init__(self, n_layers=DEPTH, stop=None, debug=()):
        self.n_layers = n_layers
        self.stop = stop
        self.debug = set(debug)
        nc = self.nc = bass.Bass("TRN2", target_bir_lowering=False)
        self.S = Sched(nc)
        dt = lambda name, shape, dtype=F32, kind="ExternalInput": nc.dram_tensor(name, shape, dtype, kind=kind)
        L = DEPTH
        self.inp = {
            "x": dt("x", [T, D]), "mem": dt("mem", [NSEQ * MEM, D]),
            "w_in": dt("w_in", [L, D, INW]), "b_gate": dt("b_gate", [L, 3 * D]),
            "diff_lambda": dt("diff_lambda", [L, 256]), "diff_subln_g": dt("diff_subln_g", [L, 128]),
            "rel_bias": dt("rel_bias", [32, 8]), "w_mem_kv": dt("w_mem_kv", [L, D, D]),
            "w_branch": dt("w_branch", [L, 3 * W, D]), "w_out": dt("w_out", [L, D, D]),
            "ln1_g": dt("ln1_g", [L, D]), "ln1_b": dt("ln1_b", [L, D]),
            "router_w": dt("router_w", [L, D, NE]), "router_b": dt("router_b", [L, NE]),
            "b_gate_up": dt("b_gate_up", [L, NE, 2 * D]), "b_down": dt("b_down", [L, NE, D]),
            "ln2_g": dt("ln2_g", [L, D]), "ln2_b": dt("ln2_b", [L, D]),
        }
        import os as _os
        self.with_moe = (stop is None) or stop.startswith("p6") or bool(_os.environ.get("FORCE_MOE"))
        if self.with_moe:
            self.inp["w_gate_up"] = dt("w_gate_up", [L, NE, D, 2 * D])
            self.inp["w_down"] = dt("w_down", [L, NE, D, D])
        for k, shp in CONST_SHAPES.items():
            self.inp["c_" + k] = dt("c_" + k, shp)
        self.out = dt("out", [T, D], F32, "ExternalOutput")

        def scr(name, shape, dtype):
            kind = "ExternalOutput" if name in self.debug else "Internal"
            return nc.dram_tensor(name, shape, dtype, kind=kind)
        self.QKT = scr("QKT", [20 * 128, T], BF16)
        self.V = scr("V", [T, 1024], BF16)
        self.GT = scr("GT", [3072, T], BF16)
        self.YT = scr("YT", [1536, T], BF16)
        self.X1 = scr("X1", [T, D], F32)
        self.X1T = scr("X1T", [D, T], BF16)
        self.CW = scr("CW", [T, NE], F32)
        self.CWT = scr("CWT", [NE, T], F32)
        self.XC = scr("XC", [T, D], F32)
        self.FD = scr("FD", [8, 384], F32)
        self.r = {k: Res(k, dram=True) for k in ("QKT", "V", "GT", "YT", "X1", "X1T", "CW", "CWT", "XC", "FD", "out")}
        self.ps = [nc.alloc_psum_tensor(f"ps{i}", [128, 512], F32)[:] for i in range(8)]
        self.ps_r = [Res(f"ps{i}", excl=True) for i in range(8)]
        self.build()

    def dump(self, name, ap, res, shape, dtype):
        if "DBG2" not in self.debug:
            return
        t = self.nc.dram_tensor(name, shape, dtype, kind="ExternalOutput")
        self.S.dma("sp", t.ap(), ap, [res], [Res(name, True)], "dbg_" + name)

    def mm(self, out, lhsT, rhs, start, stop, reads, writes, inc):
        self.S.op("pe", (lambda e: e.matmul(out, lhsT=lhsT, rhs=rhs, start=start, stop=stop)), reads, writes, inc)

    def load_const(self, name, dtype, queue=None):
        S = self.S
        shp = CONST_SHAPES[name]
        t = S.sb(shp, dtype, name)
        r = Res(name)
        q = "pool" if dtype == BF16 else "sp"
        S.dma(q, t, self.inp["c_" + name].ap(), [], [r], "c_" + name + ("b" if dtype == BF16 else "f"))
        return t, r

    def build(self):
        S = self.S
        self.ident, self.r_ident = self.load_const("ident", F32)
        self.ident_bf, self.r_ident_bf = self.load_const("ident", BF16)
        self.ones_bf, self.r_ones_bf = self.load_const("ones", BF16)
        self.eps_t = S.sb([128, 1], F32, "eps"); self.r_eps = Res("eps")
        S.op("pool", lambda e: e.memset(self.eps_t, 1e-5), [], [self.r_eps])
        self.Dhi = S.sb([128, 16, 128], BF16, "Dhi"); self.r_Dhi = Res("Dhi")
        self.b15 = S.sb([128, 8], F32, "b15"); self.r_b15 = Res("b15")
        S.persist_off = S.sb_off
        self.setup_bias()
        x_src = self.inp["x"].ap(); r_xsrc = Res("xin", dram=True)
        for l in range(self.n_layers):
            last = (l == self.n_layers - 1)
            if self.stop == f"pre{l}": break
            self.phase1(l, x_src, r_xsrc)
            if self.stop == f"p1_{l}": break
            self.phase_sb(l)
            if self.stop == f"p2_{l}": break
            self.phase_diff(l)
            if self.stop == f"p3_{l}": break
            self.phase_mem(l)
            if self.stop == f"p4_{l}": break
            self.phase5(l, x_src, r_xsrc)
            if self.stop == f"p5_{l}": break
            dst = self.out.ap() if last else self.XC.ap()
            r_dst = self.r["out"] if last else self.r["XC"]
            self.phase_moe(l, dst, r_dst)
            if self.stop == f"p6_{l}": break
            x_src = self.XC.ap(); r_xsrc = self.r["XC"]
        S.emit()

    def setup_bias(self):
        S = self.S; ps = self.ps; ps_r = self.ps_r
        S.new_phase()
        tab = S.sb([32, 8], F32, "tab"); r_tab = Res()
        oh = S.sb([32, 384], F32, "oh"); r_oh = Res()
        S.dma("sp", tab, self.inp["rel_bias"].ap(), [], [r_tab], "tab")
        S.dma("sp", oh, self.inp["c_bucket_oh"].ap(), [], [r_oh], "oh")
        anti, r_anti = self.load_const("antiid", F32)
        self.mm(ps[0][0:8, 0:384], tab, oh, True, True, [r_tab, r_oh], [ps_r[0]], True)
        ft = S.sb([8, 384], F32, "ft"); r_ft = Res()
        S.op("dve", lambda e: e.tensor_copy(out=ft, in_=ps[0][0:8, 0:384]), [ps_r[0]], [r_ft])
        S.dma("sp", self.FD.ap(), ft, [r_ft], [self.r["FD"]], "ft")
        S.dma("sp", self.b15, bass.AP(tensor=self.FD, offset=0, ap=[[0, 128], [384, 8]]), [self.r["FD"]], [self.r_b15],
              "b15", allow_slow_non_contiguous=True)
        xt = [S.sb([128, 128], F32, "xtoe") for _ in range(2)]; r_xt = [Res(), Res()]
        dm = [S.sb([128, 128], F32, "dm") for _ in range(2)]; r_dm = [Res(), Res()]
        k = 0
        for h in range(8):
            for d in range(2):
                base = 128 if d == 0 else 0
                b = k % 2
                S.dma("sp", xt[b], bass.AP(tensor=self.FD, offset=h * 384 + base, ap=[[1, 128], [1, 128]]),
                      [self.r["FD"]], [r_xt[b]], f"xtoe{b}")
                pb = 1 + b
                self.mm(ps[pb][:, 0:128], xt[b], anti, True, True, [r_xt[b], r_anti], [ps_r[pb]], True)
                S.op("dve", (lambda e, b=b, pb=pb, h=h: e.tensor_scalar(out=dm[b], in0=ps[pb][:, 0:128], scalar1=self.b15[:, h:h + 1],
                                                                 scalar2=None, op0=ALU.subtract)),
                     [ps_r[pb], self.r_b15], [r_dm[b]])
                if d == 0:
                    S.op("dve", (lambda e, b=b: e.memset(dm[b][64:128, 0:64], NEG)), [], [r_dm[b]])
                S.op("dve", (lambda e, b=b, h=h, d=d: e.tensor_copy(out=self.Dhi[:, d * 8 + h, :], in_=dm[b])), [r_dm[b]], [self.r_Dhi])
                k += 1

    def phase1(self, l, x_src, r_xsrc):
        S = self.S; ps = self.ps; ps_r = self.ps_r
        S.new_phase()
        w = S.sb([128, 8, INW], BF16, "w_in"); r_w = Res()
        w_in = self.inp["w_in"].ap()
        for c in range(8):
            S.dma("pool", w[:, c, :], w_in[l, c * 128:(c + 1) * 128, :], [], [r_w], "p1w")
        bg = S.sb([128, 24], F32, "bg"); r_bg = Res()
        S.dma("sp", bg, self.inp["b_gate"].ap()[l].rearrange("(j p) -> p j", p=128), [], [r_bg], "bg", allow_slow_non_contiguous=True)
        xt = [S.sb([128, 4, D], F32, "xt") for _ in range(2)]; r_xt = [Res(), Res()]
        xT = [S.sb([128, 8, 512], BF16, "xT") for _ in range(2)]; r_xT = [Res(), Res()]
        NST = 3
        stg = [S.sb([128, 4, 512], BF16, "stg") for _ in range(NST)]; r_stg = [Res() for _ in range(NST)]
        vst = [S.sb([128, 4, 1024], BF16, "vst") for _ in range(2)]; r_vst = [Res(), Res()]
        NT = T // 512
        qcols = [0, 512, 1536, 2048, 3072]
        def load(i):
            S.dma("sp", xt[i % 2], x_src[i * 512:(i + 1) * 512, :].rearrange("(s p) d -> p s d", p=128), [r_xsrc], [r_xt[i % 2]], f"p1x{i % 2}")
        load(0)
        gi = 0
        pbi = 0
        for i in range(NT):
            if i + 1 < NT:
                load(i + 1)
            b = i % 2
            t0 = i * 512
            for c in range(8):
                pb = pbi % 8; pbi += 1
                for s in range(4):
                    S.op("pe", (lambda e, pb=pb, s=s, c=c, b=b: e.transpose(out=ps[pb][:, s * 128:(s + 1) * 128], in_=xt[b][:, s, c * 128:(c + 1) * 128],
                                                                   identity=self.ident)),
                         [r_xt[b], self.r_ident], [ps_r[pb]], inc=(s == 3))
                if c % 2 == 0:
                    S.op("dve", (lambda e, pb=pb, c=c, b=b: e.tensor_copy(out=xT[b][:, c, :], in_=ps[pb])), [ps_r[pb]], [r_xT[b]])
                else:
                    S.op("act", (lambda e, pb=pb, c=c, b=b: e.copy(out=xT[b][:, c, :], in_=ps[pb])), [ps_r[pb]], [r_xT[b]])
            for g in range(11):
                sl = gi % NST; gi += 1
                for m in range(4):
                    if g < 5:
                        col = qcols[g] + m * 128
                    else:
                        col = 3584 + ((g - 5) * 4 + m) * 128
                    pb = pbi % 8; pbi += 1
                    for c in range(8):
                        self.mm(ps[pb], w[:, c, col:col + 128], xT[b][:, c, :], c == 0, c == 7, [r_w, r_xT[b]], [ps_r[pb]], c == 7)
                    if g < 5:
                        scale = 0.125 if g in (0, 2) else 1.0
                        S.op("dve", (lambda e, pb=pb, sl=sl, m=m, scale=scale: e.tensor_scalar(out=stg[sl][:, m, :], in0=ps[pb], scalar1=scale, scalar2=None,
                                                                                         op0=ALU.mult)),
                             [ps_r[pb]], [r_stg[sl]])
                    else:
                        j = (g - 5) * 4 + m
                        S.op("act", (lambda e, pb=pb, sl=sl, m=m, j=j: e.activation(out=stg[sl][:, m, :], in_=ps[pb], func=AF.Sigmoid, bias=bg[:, j:j + 1], scale=1.0)),
                             [ps_r[pb], r_bg], [r_stg[sl]])
                if g < 5:
                    dst = self.QKT.ap()[g * 512:(g + 1) * 512, t0:t0 + 512].rearrange("(c p) t -> p c t", p=128)
                    S.dma("sp", dst, stg[sl], [r_stg[sl]], [self.r["QKT"]], f"p1s{sl}")
                else:
                    dst = self.GT.ap()[(g - 5) * 512:(g - 4) * 512, t0:t0 + 512].rearrange("(c p) t -> p c t", p=128)
                    S.dma("sp", dst, stg[sl], [r_stg[sl]], [self.r["GT"]], f"p1s{sl}")
            vb = i % 2
            k = 0
            for s in range(4):
                for hv in range(2):
                    col = 1024 if hv == 0 else 2560
                    pb = pbi % 8; pbi += 1
                    for c in range(8):
                        self.mm(ps[pb], xT[b][:, c, s * 128:(s + 1) * 128], w[:, c, col:col + 512], c == 0, c == 7, [r_w, r_xT[b]], [ps_r[pb]], c == 7)
                    if k % 2 == 0:
                        S.op("dve", (lambda e, pb=pb, s=s, hv=hv, vb=vb: e.tensor_copy(out=vst[vb][:, s, hv * 512:(hv + 1) * 512], in_=ps[pb])), [ps_r[pb]], [r_vst[vb]])
                    else:
                        S.op("act", (lambda e, pb=pb, s=s, hv=hv, vb=vb: e.copy(out=vst[vb][:, s, hv * 512:(hv + 1) * 512], in_=ps[pb])), [ps_r[pb]], [r_vst[vb]])
                    k += 1
            S.dma("sp", self.V.ap()[t0:t0 + 512, :].rearrange("(s p) f -> p s f", p=128), vst[vb], [r_vst[vb]], [self.r["V"]], f"p1v{vb}")

    def phase_sb(self, l):
        S = self.S; ps = self.ps; ps_r = self.ps_r
        S.new_phase()
        uin, r_uin = self.load_const("uincl_neg", BF16)
        oneg, r_oneg = self.load_const("ones_neg", BF16)
        msk, r_msk = self.load_const("sbmask", BF16)
        idb, r_idb = self.ident_bf, self.r_ident_bf
        QT = [[S.sb([128, SEQ], BF16, "QT") for _ in range(2)] for _ in range(2)]
        r_QT = [[Res(), Res()], [Res(), Res()]]
        for hh in range(2):
            for sl_ in range(2):
                z0 = (1 - hh) * 64
                S.op("pool", (lambda e, hh=hh, sl_=sl_, z0=z0: e.memset(QT[hh][sl_][z0:z0 + 64, :], 0.0)), [], [r_QT[hh][sl_]])
        KT = [S.sb([128, SEQ], BF16, "KT") for _ in range(2)]; r_KT = [Res(), Res()]
        Vp = [S.sb([128, 32, 128], BF16, "Vp") for _ in range(2)]; r_Vp = [Res(), Res()]
        ost = [S.sb([128, SEQ], BF16, "ost") for _ in range(1)] * 2; r_ost = [Res()] * 2
        ebuf = [S.sb([128, 512], F32, "e") for _ in range(2)]; r_e = [Res(), Res()]
        sp = [S.sb([128, 512], BF16, "sp") for _ in range(2)]; r_sp = [Res(), Res()]
        aT = [S.sb([128, 512], BF16, "aT") for _ in range(2)]; r_aT = [Res(), Res()]
        lacc = S.sb([128, 512], F32, "lacc"); r_lacc = Res()
        lbf = [S.sb([128, 512], BF16, "lbf") for _ in range(3)]; r_lbf = [Res(), Res(), Res()]
        pairs = [(b, hp) for b in range(NSEQ) for hp in range(4)]
        import os
        if "DBG" in self.debug:
            pairs = pairs[:int(os.environ.get("DBG_PAIRS", "1"))]

        def load(pi):
            b, hp = pairs[pi]
            sl = pi % 2
            for hh in range(2):
                S.dma("sp", QT[hh][sl][hh * 64:(hh + 1) * 64, :], self.QKT.ap()[hp * 128 + hh * 64:hp * 128 + (hh + 1) * 64, b * SEQ:(b + 1) * SEQ],
                      [self.r["QKT"]], [r_QT[hh][sl]], f"sbq{hh}{sl}")
            S.dma("sp", KT[sl], self.QKT.ap()[512 + hp * 128:512 + (hp + 1) * 128, b * SEQ:(b + 1) * SEQ], [self.r["QKT"]], [r_KT[sl]], f"sbk{sl}")
            S.dma("sp", Vp[sl], self.V.ap()[b * SEQ:(b + 1) * SEQ, hp * 128:(hp + 1) * 128].rearrange("(k p) f -> p k f", p=128),
                  [self.r["V"]], [r_Vp[sl]], f"sbv{sl}")
        load(0)
        for pi, (b, hp) in enumerate(pairs):
            if pi + 1 < len(pairs):
                load(pi + 1)
            sl = pi % 2
            blocks = []
            for hh in range(2):
                for qb in range(8):
                    kbs = list(range(4 * qb + 3, -1, -1))
                    for idx, kb in enumerate(kbs):
                        blocks.append((hh, qb, kb, idx, idx == len(kbs) - 1))
            if "DBG" in self.debug:
                blocks = blocks[:int(os.environ.get("DBG_BLOCKS", "12"))]
            n = len(blocks)

            def zmm(bank, r_bank, hh, qb, kb, start):
                base = hh * 64
                j = kb - 4 * qb
                c0 = 128 * j if j > 0 else 0
                diag = j >= 0
                self.mm(bank[:, c0:512], KT[sl][:, kb * 128:(kb + 1) * 128], QT[hh][sl][:, qb * 512 + c0:(qb + 1) * 512],
                        start, not diag, [r_KT[sl], r_QT[hh][sl]], [r_bank], not diag)
                if diag:
                    self.mm(bank[:, c0:c0 + 128], idb, msk, False, True, [r_idb, r_msk], [r_bank], True)
                return c0

            def stageA(k):
                hh, qb, kb, idx, lastk = blocks[k]
                p = k % 2
                c0 = zmm(ps[p], ps_r[p], hh, qb, kb, True)
                S.op("act", (lambda e: e.activation(out=ebuf[p][:, c0:512], in_=ps[p][:, c0:512], func=AF.Exp)), [ps_r[p]], [r_e[p]])
                S.op("act", (lambda e: e.activation(out=sp[p][:, c0:512], in_=ebuf[p][:, c0:512], func=AF.Ln, bias=1.0, scale=1.0)), [r_e[p]], [r_sp[p]])
                if k < 6:
                    self.dump(f"d_e{k}", ebuf[p], r_e[p], [128, 512], F32)
                    self.dump(f"d_sp{k}", sp[p], r_sp[p], [128, 512], BF16)
                if not lastk:
                    if idx == 0:
                        S.op("dve", (lambda e: e.memset(lacc, 0.0)), [], [r_lacc])
                    S.op("dve", (lambda e: e.tensor_tensor(out=lacc[:, c0:512], in0=lacc[:, c0:512], in1=sp[p][:, c0:512], op=ALU.add)), [r_sp[p], r_lacc], [r_lacc])
                    q = (k + 1) % 3
                    S.op("dve", (lambda e: e.tensor_copy(out=lbf[q], in_=lacc)), [r_lacc], [r_lbf[q]])

            def stageB(k):
                hh, qb, kb, idx, lastk = blocks[k]
                p = k % 2
                bank = ps[2 + p]; r_bank = ps_r[2 + p]
                j = kb - 4 * qb
                c0 = 128 * j if j > 0 else 0
                self.mm(bank[:, c0:512], uin, sp[p][:, c0:512], True, False, [r_uin, r_sp[p]], [r_bank], False)
                if idx > 0:
                    q = k % 3
                    self.mm(bank[:, c0:512], oneg, lbf[q][:, c0:512], False, False, [r_oneg, r_lbf[q]], [r_bank], False)
                zmm(bank, r_bank, hh, qb, kb, False)
                S.op("act", (lambda e: e.activation(out=aT[p][:, c0:512], in_=bank[:, c0:512], func=AF.Exp)), [r_bank], [r_aT[p]])
                if k < 6:
                    self.dump(f"d_a{k}", aT[p], r_aT[p], [128, 512], BF16)
                    if idx > 0:
                        self.dump(f"d_l{k}", lbf[k % 3], r_lbf[k % 3], [128, 512], BF16)

            def stageC(k):
                hh, qb, kb, idx, lastk = blocks[k]
                p = k % 2
                base = hh * 64
                ob = 4 + (qb % 2)
                j = kb - 4 * qb
                c0 = 128 * j if j > 0 else 0
                self.mm(ps[ob][:, c0:512], Vp[sl][:, kb, :], aT[p][:, c0:512], idx == 0, lastk,
                        [r_Vp[sl], r_aT[p]], [ps_r[ob]], True)
                if lastk:
                    S.op("dve", (lambda e: e.tensor_copy(out=ost[sl][base:base + 64, qb * 512:(qb + 1) * 512], in_=ps[ob][base:base + 64, :])),
                         [ps_r[ob]], [r_ost[sl]])
            for step in range(n + 2):
                if step < n: stageA(step)
                if 0 <= step - 1 < n: stageB(step - 1)
                if 0 <= step - 2 < n: stageC(step - 2)
            S.dma("sp", self.YT.ap()[hp * 128:(hp + 1) * 128, b * SEQ:(b + 1) * SEQ], ost[sl], [r_ost[sl]], [self.r["YT"]], f"sbo{sl}")

    def phase_diff(self, l):
        S = self.S; ps = self.ps; ps_r = self.ps_r
        S.new_phase()
        import math, os
        lam_init = 0.8 - 0.6 * math.exp(-0.3 * l)
        idb, r_idb = self.ident_bf, self.r_ident_bf
        onesb, r_onesb = self.ones_bf, self.r_ones_bf
        dl = S.sb([128, 256], F32, "dl"); r_dl = Res()
        S.dma("sp", dl, bass.AP(tensor=self.inp["diff_lambda"], offset=l * 256, ap=[[0, 128], [1, 256]]), [], [r_dl], "dl")
        pr = S.sb([128, 2, 64], F32, "pr"); r_pr = Res()
        S.op("dve", lambda e: e.tensor_tensor(out=pr[:, 0, :], in0=dl[:, 0:64], in1=dl[:, 64:128], op=ALU.mult), [r_dl], [r_pr])
        S.op("dve", lambda e: e.tensor_tensor(out=pr[:, 1, :], in0=dl[:, 128:192], in1=dl[:, 192:256], op=ALU.mult), [r_dl], [r_pr])
        sm = S.sb([128, 4], F32, "sm"); r_sm = Res()
        S.op("dve", lambda e: e.tensor_reduce(out=sm[:, 0:2], in_=pr, axis=AX.X, op=ALU.add), [r_pr], [r_sm])
        S.op("act", lambda e: e.activation(out=sm[:, 2:4], in_=sm[:, 0:2], func=AF.Exp), [r_sm], [r_sm])
        nlam = S.sb([128, 1], F32, "nlam"); r_nlam = Res()
        S.op("dve", lambda e: e.scalar_tensor_tensor(out=nlam, in0=sm[:, 3:4], scalar=-lam_init, in1=sm[:, 2:3], op0=ALU.add, op1=ALU.subtract), [r_sm], [r_nlam])
        gp = S.sb([128, 1], F32, "gp"); r_gp = Res()
        S.dma("sp", gp, self.inp["diff_subln_g"].ap()[l].rearrange("(p o) -> p o", o=1), [], [r_gp], "gp")
        S.op("dve", lambda e: e.tensor_scalar(out=gp, in0=gp, scalar1=(1.0 - lam_init), scalar2=None, op0=ALU.mult), [r_gp], [r_gp])
        QT = [[S.sb([128, SEQ], BF16, "dQT") for _ in range(2)] for _ in range(2)]
        r_QT = [[Res(), Res()], [Res(), Res()]]
        for hh in range(2):
            for sl_ in range(2):
                z0 = (1 - hh) * 64
                S.op("pool", (lambda e, hh=hh, sl_=sl_, z0=z0: e.memset(QT[hh][sl_][z0:z0 + 64, :], 0.0)), [], [r_QT[hh][sl_]])
        KT = [S.sb([128, SEQ], BF16, "dKT") for _ in range(2)]; r_KT = [Res(), Res()]
        Vp = [S.sb([128, 32, 128], BF16, "dVp") for _ in range(2)]; r_Vp = [Res(), Res()]
        yst = S.sb([128, SEQ], BF16, "yst"); r_yst = Res()
        PT = [S.sb([128, 512], BF16, "PT") for _ in range(2)]; r_PT = [Res(), Res()]
        rr = [S.sb([128, 512], F32, "rr") for _ in range(2)]; r_rr = [Res(), Res()]
        oo = S.sb([128, 512], F32, "oo"); r_oo = Res()
        t2 = S.sb([128, 512], F32, "t2"); r_t2 = Res()
        sq = S.sb([128, 512], BF16, "sq"); r_sq = Res()
        rs = S.sb([128, 512], F32, "rs"); r_rs = Res()
        pairs = [(b, dh) for b in range(NSEQ) for dh in range(4)]
        if "DBG" in self.debug:
            pairs = pairs[:int(os.environ.get("DBG_PAIRS", "1"))]

        def load(pi):
            b, dh = pairs[pi]
            sl = pi % 2
            for hh in range(2):
                S.dma("sp", QT[hh][sl][hh * 64:(hh + 1) * 64, :], self.QKT.ap()[1024 + dh * 128 + hh * 64:1024 + dh * 128 + (hh + 1) * 64, b * SEQ:(b + 1) * SEQ],
                      [self.r["QKT"]], [r_QT[hh][sl]], f"dfq{hh}{sl}")
            S.dma("sp", KT[sl], self.QKT.ap()[1536 + dh * 128:1536 + (dh + 1) * 128, b * SEQ:(b + 1) * SEQ], [self.r["QKT"]], [r_KT[sl]], f"dfk{sl}")
            S.dma("sp", Vp[sl], self.V.ap()[b * SEQ:(b + 1) * SEQ, 512 + dh * 128:512 + (dh + 1) * 128].rearrange("(k p) f -> p k f", p=128),
                  [self.r["V"]], [r_Vp[sl]], f"dfv{sl}")
        load(0)
        kglob = 0
        for pi, (b, dh) in enumerate(pairs):
            if pi + 1 < len(pairs):
                load(pi + 1)
            sl = pi % 2
            for qb in range(8):
                for mp in range(2):
                    h = 2 * dh + mp
                    kbs = list(range(4 * qb + 3, -1, -1))
                    n = len(kbs)
                    ob = 2 + mp; db = 4 + mp

                    def stageA(idx, kg):
                        kb = kbs[idx]
                        p = kg % 2
                        j = kb - 4 * qb
                        c0 = 128 * j if j > 0 else 0
                        extra = []
                        if 0 <= j <= 3:
                            extra.append((j, 0))
                        if 0 <= j + 1 <= 3:
                            extra.append((j + 1, 1))
                        self.mm(ps[p][:, c0:512], KT[sl][:, kb * 128:(kb + 1) * 128], QT[mp][sl][:, qb * 512 + c0:(qb + 1) * 512],
                                True, len(extra) == 0, [r_KT[sl], r_QT[mp][sl]], [ps_r[p]], len(extra) == 0)
                        for ei, (qs, d) in enumerate(extra):
                            lastx = ei == len(extra) - 1
                            self.mm(ps[p][:, qs * 128:(qs + 1) * 128], idb, self.Dhi[:, d * 8 + h, :], False, lastx, [r_idb, self.r_Dhi], [ps_r[p]], lastx)
                        S.op("act", (lambda e: e.activation(out=PT[p][:, c0:512], in_=ps[p][:, c0:512], func=AF.Exp, bias=self.b15[:, h:h + 1], scale=1.0)),
                             [ps_r[p], self.r_b15], [r_PT[p]])

                    def stageB(idx, kg):
                        kb = kbs[idx]
                        p = kg % 2
                        j = kb - 4 * qb
                        c0 = 128 * j if j > 0 else 0
                        self.mm(ps[ob][:, c0:512], Vp[sl][:, kb, :], PT[p][:, c0:512], idx == 0, idx == n - 1, [r_Vp[sl], r_PT[p]], [ps_r[ob]], True)
                        self.mm(ps[db][:, c0:512], onesb, PT[p][:, c0:512], idx == 0, idx == n - 1, [r_onesb, r_PT[p]], [ps_r[db]], True)
                    for step in range(n + 1):
                        if step < n: stageA(step, kglob + step)
                        if step >= 1: stageB(step - 1, kglob + step - 1)
                    kglob += n
                    S.op("dve", (lambda e, mp=mp, db=db: e.reciprocal(out=rr[mp], in_=ps[db])), [ps_r[db]], [r_rr[mp]])
                    if mp == 0:
                        S.op("dve", (lambda e, ob=ob: e.tensor_tensor(out=oo, in0=ps[ob], in1=rr[0], op=ALU.mult)), [ps_r[ob], r_rr[0]], [r_oo])
                    else:
                        S.op("dve", (lambda e, ob=ob: e.tensor_tensor(out=t2, in0=ps[ob], in1=rr[1], op=ALU.mult)), [ps_r[ob], r_rr[1]], [r_t2])
                S.op("dve", lambda e: e.scalar_tensor_tensor(out=oo, in0=t2, scalar=nlam[:, 0:1], in1=oo, op0=ALU.mult, op1=ALU.add), [r_t2, r_nlam, r_oo], [r_oo])
                S.op("pool", lambda e: e.tensor_tensor(out=sq, in0=oo, in1=oo, op=ALU.mult), [r_oo], [r_sq])
                self.mm(ps[6], onesb, sq, True, True, [r_onesb, r_sq], [ps_r[6]], True)
                S.op("act", lambda e: e.activation(out=rs, in_=ps[6], func=AF.Sqrt, bias=self.eps_t[:, 0:1], scale=1.0 / 128.0), [ps_r[6], self.r_eps], [r_rs])
                S.op("dve", lambda e: e.reciprocal(out=rs, in_=rs), [r_rs], [r_rs])
                S.op("dve", (lambda e, qb=qb: e.scalar_tensor_tensor(out=yst[:, qb * 512:(qb + 1) * 512], in0=oo, scalar=gp[:, 0:1], in1=rs, op0=ALU.mult, op1=ALU.mult)),
                     [r_oo, r_gp, r_rs], [r_yst])
            S.dma("sp", self.YT.ap()[512 + dh * 128:512 + (dh + 1) * 128, b * SEQ:(b + 1) * SEQ], yst, [r_yst], [self.r["YT"]], "dfo")

    def phase_mem(self, l):
        S = self.S; ps = self.ps; ps_r = self.ps_r
        S.new_phase()
        onesb, r_onesb = self.ones_bf, self.r_ones_bf
        wkv = S.sb([128, 8, D], BF16, "wkv"); r_wkv = Res()
        for c in range(8):
            S.dma("pool", wkv[:, c, :], self.inp["w_mem_kv"].ap()[l, c * 128:(c + 1) * 128, :], [], [r_wkv], "wkv")
        mt = S.sb([128, 2, D], F32, "mt"); r_mt = Res()
        mT = S.sb([128, 8, MEM], BF16, "mT"); r_mT = Res()
        mKT = S.sb([128, 4, MEM], BF16, "mKT"); r_mKT = Res()
        mV = S.sb([128, 2, W], BF16, "mV"); r_mV = Res()
        QT = [S.sb([128, SEQ], BF16, "mQT") for _ in range(2)]; r_QT = [Res(), Res()]
        yst = [S.sb([128, SEQ], BF16, "myst") for _ in range(2)]; r_yst = [Res(), Res()]
        PT = [S.sb([128, 2, 512], BF16, "mPT") for _ in range(2)]; r_PT = [Res(), Res()]
        rr = S.sb([128, 512], F32, "mrr"); r_rr = Res()
        scale = 128.0 ** -0.5
        jobs = [(b, h) for b in range(NSEQ) for h in range(4)]

        def loadq(ji):
            b, h = jobs[ji]
            S.dma("sp", QT[ji % 2], self.QKT.ap()[2048 + h * 128:2048 + (h + 1) * 128, b * SEQ:(b + 1) * SEQ], [self.r["QKT"]], [r_QT[ji % 2]], f"mq{ji % 2}")
        loadq(0)
        ji = 0
        for b in range(NSEQ):
            S.dma("sp", mt, self.inp["mem"].ap()[b * MEM:(b + 1) * MEM, :].rearrange("(s p) d -> p s d", p=128), [], [r_mt], "mt")
            for c in range(8):
                pb = c % 2
                for s in range(2):
                    S.op("pe", (lambda e, pb=pb, s=s, c=c: e.transpose(out=ps[pb][:, s * 128:(s + 1) * 128], in_=mt[:, s, c * 128:(c + 1) * 128], identity=self.ident)),
                         [r_mt, self.r_ident], [ps_r[pb]], inc=(s == 1))
                S.op("dve", (lambda e, pb=pb, c=c: e.tensor_copy(out=mT[:, c, :], in_=ps[pb][:, 0:256])), [ps_r[pb]], [r_mT])
            for h in range(4):
                pb = 2 + h % 2
                for c in range(8):
                    self.mm(ps[pb][:, 0:256], wkv[:, c, h * 128:(h + 1) * 128], mT[:, c, :], c == 0, c == 7, [r_wkv, r_mT], [ps_r[pb]], c == 7)
                S.op("dve", (lambda e, pb=pb, h=h: e.tensor_copy(out=mKT[:, h, :], in_=ps[pb][:, 0:256])), [ps_r[pb]], [r_mKT])
            for mb in range(2):
                pb = 4 + mb
                for c in range(8):
                    self.mm(ps[pb], mT[:, c, mb * 128:(mb + 1) * 128], wkv[:, c, 512:1024], c == 0, c == 7, [r_wkv, r_mT], [ps_r[pb]], c == 7)
                S.op("dve", (lambda e, pb=pb, mb=mb: e.tensor_copy(out=mV[:, mb, :], in_=ps[pb])), [ps_r[pb]], [r_mV])
            for h in range(4):
                if ji + 1 < len(jobs):
                    loadq(ji + 1)
                sl = ji % 2
                for qt in range(8):
                    p = qt % 2
                    for mb in range(2):
                        zb = 0 + mb if p == 0 else 2 + mb
                        self.mm(ps[zb], mKT[:, h, mb * 128:(mb + 1) * 128], QT[sl][:, qt * 512:(qt + 1) * 512], True, True, [r_mKT, r_QT[sl]], [ps_r[zb]], True)
                        S.op("act", (lambda e, zb=zb, p=p, mb=mb: e.activation(out=PT[p][:, mb, :], in_=ps[zb], func=AF.Exp, scale=scale)), [ps_r[zb]], [r_PT[p]])
                    ob = 4 + p; db = 6 + p
                    for mb in range(2):
                        self.mm(ps[ob], mV[:, mb, h * 128:(h + 1) * 128], PT[p][:, mb, :], mb == 0, mb == 1, [r_mV, r_PT[p]], [ps_r[ob]], mb == 1)
                    for mb in range(2):
                        self.mm(ps[db], onesb, PT[p][:, mb, :], mb == 0, mb == 1, [r_onesb, r_PT[p]], [ps_r[db]], mb == 1)
                    S.op("dve", (lambda e, db=db: e.reciprocal(out=rr, in_=ps[db])), [ps_r[db]], [r_rr])
                    S.op("dve", (lambda e, ob=ob, sl=sl, qt=qt: e.tensor_tensor(out=yst[sl][:, qt * 512:(qt + 1) * 512], in0=ps[ob], in1=rr, op=ALU.mult)),
                         [ps_r[ob], r_rr], [r_yst[sl]])
                S.dma("sp", self.YT.ap()[1024 + h * 128:1024 + (h + 1) * 128, b * SEQ:(b + 1) * SEQ], yst[sl], [r_yst[sl]], [self.r["YT"]], f"mo{sl}")
                ji += 1

    def layer_norm(self, zt, r_z, g_t, b_t, r_gb, tmp):
        S = self.S
        st, mv, rstd, r_st = tmp
        for j in range(2):
            S.op("dve", (lambda e, j=j: e.bn_stats(out=st[:, j, :], in_=zt[:, j * 512:(j + 1) * 512])), [r_z], [r_st])
        S.op("dve", lambda e: e.bn_aggr(out=mv, in_=st), [r_st], [r_st])
        S.op("act", lambda e: e.activation(out=rstd, in_=mv[:, 1:2], func=AF.Sqrt, bias=self.eps_t[:, 0:1], scale=1.0), [r_st, self.r_eps], [r_st])
        S.op("dve", lambda e: e.reciprocal(out=rstd, in_=rstd), [r_st], [r_st])
        S.op("dve", lambda e: e.tensor_scalar(out=zt, in0=zt, scalar1=mv[:, 0:1], scalar2=rstd[:, 0:1], op0=ALU.subtract, op1=ALU.mult), [r_z, r_st], [r_z])
        S.op("pool", lambda e: e.tensor_tensor(out=zt, in0=zt, in1=g_t, op=ALU.mult), [r_z, r_gb], [r_z])
        S.op("pool", lambda e: e.tensor_tensor(out=zt, in0=zt, in1=b_t, op=ALU.add), [r_z, r_gb], [r_z])

    def ln_tmp(self):
        S = self.S
        return (S.sb([128, 2, 6], F32, "lnst"), S.sb([128, 2], F32, "lnmv"), S.sb([128, 1], F32, "lnrs"), Res())

    def bcast_row(self, src_tensor, offset, n, name):
        S = self.S
        t = S.sb([128, n], F32, name); r = Res()
        S.dma("sp", t, bass.AP(tensor=src_tensor, offset=offset, ap=[[0, 128], [1, n]]), [], [r], "bc_" + name)
        return t, r

    def phase5(self, l, x_src, r_xsrc):
        S = self.S; ps = self.ps; ps_r = self.ps_r
        S.new_phase()
        wb = S.sb([128, 12, D], BF16, "wb"); r_wb = Res()
        for c in range(0, 12, 4):
            S.dma("pool", wb[:, c:c + 4, :], self.inp["w_branch"].ap()[l, c * 128:(c + 4) * 128, :].rearrange("(c p) n -> p c n", p=128), [], [r_wb], "wb")
        wo = S.sb([128, 8, D], BF16, "wo"); r_wo = Res()
        for c in range(0, 8, 4):
            S.dma("pool", wo[:, c:c + 4, :], self.inp["w_out"].ap()[l, c * 128:(c + 4) * 128, :].rearrange("(c p) n -> p c n", p=128), [], [r_wo], "wo")
        rw = S.sb([128, 8, NE], F32, "rw"); r_rw = Res()
        S.dma("sp", rw, self.inp["router_w"].ap()[l].rearrange("(c p) e -> p c e", p=128), [], [r_rw], "rw")
        rb, r_rb = self.bcast_row(self.inp["router_b"], l * NE, NE, "rb")
        g1, r_g1 = self.bcast_row(self.inp["ln1_g"], l * D, D, "g1")
        b1, r_b1 = self.bcast_row(self.inp["ln1_b"], l * D, D, "b1")
        r_gb = Res()
        S.op("pool", lambda e: e.tensor_copy(out=g1[:, 0:1], in_=g1[:, 0:1]), [r_g1, r_b1], [r_gb])
        yt = [S.sb([128, 12, 512], BF16, "yt") for _ in range(2)]; r_yt = [Res(), Res()]
        gt = S.sb([128, 24, 512], BF16, "gt"); r_gt = Res()
        xt = [S.sb([128, 4, D], F32, "xt5") for _ in range(2)]; r_xt = [Res(), Res()]
        mT = S.sb([128, 8, 512], BF16, "mT"); r_mT = Res()
        mt = [S.sb([128, 512], F32, "mtmp") for _ in range(3)]; r_mt = [Res(), Res(), Res()]
        x1Tb = S.sb([128, 8, 512], BF16, "x1Tb"); r_x1Tb = Res()
        x1Tf = S.sb([128, 8, 512], F32, "x1Tf"); r_x1Tf = Res()
        lg = S.sb([128, NE], F32, "lg"); r_lg = Res()
        t8 = S.sb([128, 16], F32, "t8"); r_t8 = Res()
        ee = S.sb([128, NE], F32, "ee"); r_ee = Res()
        cw = S.sb([128, 4, NE], F32, "cw5"); r_cw = Res()
        cwT = S.sb([NE, 512], F32, "cwT5"); r_cwT = Res()
        lnt = self.ln_tmp()
        NT = T // 512
        if "DBG" in self.debug:
            import os
            NT = int(os.environ.get("DBG_TILES", "2"))

        def load(i):
            t0 = i * 512
            S.dma("sp", yt[i % 2], self.YT.ap()[:, t0:t0 + 512].rearrange("(c p) t -> p c t", p=128), [self.r["YT"]], [r_yt[i % 2]], f"p5y{i % 2}")
            S.dma("sp", xt[i % 2], x_src[t0:t0 + 512, :].rearrange("(s p) d -> p s d", p=128), [r_xsrc], [r_xt[i % 2]], f"p5x{i % 2}")
        load(0)
        pbi = 0
        for i in range(NT):
            t0 = i * 512
            b = i % 2
            S.dma("sp", gt, self.GT.ap()[:, t0:t0 + 512].rearrange("(c p) t -> p c t", p=128), [self.r["GT"]], [r_gt], "p5g")
            if i + 1 < NT:
                load(i + 1)
            for f in range(8):
                banks = []
                for br in range(3):
                    pb = pbi % 8; pbi += 1
                    banks.append(pb)
                    for k in range(4):
                        self.mm(ps[pb], wb[:, br * 4 + k, f * 128:(f + 1) * 128], yt[b][:, br * 4 + k, :], k == 0, k == 3, [r_wb, r_yt[b]], [ps_r[pb]], k == 3)
                for br in range(3):
                    pb = banks[br]
                    S.op("dve", (lambda e, pb=pb, br=br, f=f: e.tensor_tensor(out=mt[br], in0=ps[pb], in1=gt[:, br * 8 + f, :], op=ALU.mult)),
                         [ps_r[pb], r_gt], [r_mt[br]])
                S.op("pool", lambda e: e.tensor_tensor(out=mt[0], in0=mt[0], in1=mt[1], op=ALU.add), [r_mt[0], r_mt[1]], [r_mt[0]])
                S.op("pool", (lambda e, f=f: e.tensor_tensor(out=mT[:, f, :], in0=mt[0], in1=mt[2], op=ALU.add)), [r_mt[0], r_mt[2]], [r_mT])
            if "DBG" in self.debug and i == 0 and l == 0:
                self.dump2("d_mT", mT, r_mT, [128, 8, 512], BF16)
            for s in range(4):
                for hf in range(2):
                    pb = pbi % 8; pbi += 1
                    for f in range(8):
                        self.mm(ps[pb], mT[:, f, s * 128:(s + 1) * 128], wo[:, f, hf * 512:(hf + 1) * 512], f == 0, f == 7, [r_mT, r_wo], [ps_r[pb]], f == 7)
                    S.op("dve", (lambda e, pb=pb, s=s, hf=hf, b=b: e.scalar_tensor_tensor(out=xt[b][:, s, hf * 512:(hf + 1) * 512], in0=xt[b][:, s, hf * 512:(hf + 1) * 512],
                                                                                     scalar=ALPHA, in1=ps[pb], op0=ALU.mult, op1=ALU.add)),
                         [ps_r[pb], r_xt[b]], [r_xt[b]])
                self.layer_norm(xt[b][:, s, :], r_xt[b], g1, b1, r_gb, lnt)
            S.dma("sp", self.X1.ap()[t0:t0 + 512, :].rearrange("(s p) d -> p s d", p=128), xt[b], [r_xt[b]], [self.r["X1"]], f"p5o{b}")
            for c in range(8):
                pb = pbi % 8; pbi += 1
                for s in range(4):
                    S.op("pe", (lambda e, pb=pb, s=s, c=c, b=b: e.transpose(out=ps[pb][:, s * 128:(s + 1) * 128], in_=xt[b][:, s, c * 128:(c + 1) * 128],
                                                                   identity=self.ident)),
                         [r_xt[b], self.r_ident], [ps_r[pb]], inc=(s == 3))
                S.op("dve", (lambda e, pb=pb, c=c: e.tensor_copy(out=x1Tf[:, c, :], in_=ps[pb])), [ps_r[pb]], [r_x1Tf])
                S.op("act", (lambda e, c=c: e.copy(out=x1Tb[:, c, :], in_=x1Tf[:, c, :])), [r_x1Tf], [r_x1Tb])
            S.dma("sp", self.X1T.ap()[:, t0:t0 + 512].rearrange("(c p) t -> p c t", p=128), x1Tb, [r_x1Tb], [self.r["X1T"]], "p5t")
            for s in range(4):
                pb = pbi % 8; pbi += 1
                for c in range(8):
                    self.mm(ps[pb][:, 0:NE], x1Tf[:, c, s * 128:(s + 1) * 128], rw[:, c, :], c == 0, c == 7, [r_x1Tf, r_rw], [ps_r[pb]], c == 7)
                S.op("dve", (lambda e, pb=pb: e.tensor_tensor(out=lg, in0=ps[pb][:, 0:NE], in1=rb, op=ALU.add)), [ps_r[pb], r_rb], [r_lg])
                S.op("dve", lambda e: e.max(out=t8[:, 0:8], in_=lg), [r_lg], [r_t8])
                S.op("dve", lambda e: e.tensor_scalar(out=t8[:, 8:9], in0=t8[:, 0:1], scalar1=-1.0, scalar2=None, op0=ALU.mult), [r_t8], [r_t8])
                S.op("act", lambda e: e.activation(out=ee, in_=lg, func=AF.Exp, bias=t8[:, 8:9], scale=1.0), [r_lg, r_t8], [r_ee])
                S.op("dve", lambda e: e.tensor_scalar(out=lg, in0=lg, scalar1=t8[:, 3:4], scalar2=None, op0=ALU.is_ge), [r_lg, r_t8], [r_lg])
                S.op("dve", lambda e: e.tensor_tensor(out=ee, in0=ee, in1=lg, op=ALU.mult), [r_ee, r_lg], [r_ee])
                S.op("dve", lambda e: e.tensor_reduce(out=t8[:, 9:10], in_=ee, axis=AX.X, op=ALU.add), [r_ee], [r_t8])
                S.op("dve", lambda e: e.reciprocal(out=t8[:, 10:11], in_=t8[:, 9:10]), [r_t8], [r_t8])
                S.op("dve", (lambda e, s=s: e.tensor_scalar(out=cw[:, s, :], in0=ee, scalar1=t8[:, 10:11], scalar2=None, op0=ALU.mult)), [r_ee, r_t8], [r_cw])
                pb2 = pbi % 8; pbi += 1
                self.mm(ps[pb2][0:NE, 0:128], cw[:, s, :], self.ident, True, True, [r_cw, self.r_ident], [ps_r[pb2]], True)
                S.op("dve", (lambda e, pb2=pb2, s=s: e.tensor_copy(out=cwT[:, s * 128:(s + 1) * 128], in_=ps[pb2][0:NE, 0:128])), [ps_r[pb2]], [r_cwT])
            S.dma("sp", self.CW.ap()[t0:t0 + 512, :].rearrange("(s p) e -> p s e", p=128), cw, [r_cw], [self.r["CW"]], "p5c")
            S.dma("sp", self.CWT.ap()[:, t0:t0 + 512], cwT, [r_cwT], [self.r["CWT"]], "p5ct")

    def dump2(self, name, ap, res, shape, dtype):
        t = self.nc.dram_tensor(name, shape, dtype, kind="ExternalOutput")
        self.S.dma("sp", t.ap(), ap, [res], [Res(name, True)], "dbg_" + name)

    def phase_moe(self, l, dst, r_dst):
        S = self.S; ps = self.ps; ps_r = self.ps_r
        S.new_phase()
        import os
        NB = 1024
        NSUB = NB // 128
        NTL = NB // 512
        wgu_d = self.inp["w_gate_up"].ap(); wd_d = self.inp["w_down"].ap()
        braw = S.sb([NE, 2 * D], F32, "braw"); r_braw = Res()
        S.dma("sp", braw, self.inp["b_gate_up"].ap()[l], [], [r_braw], "braw")
        bguT = S.sb([128, 16, NE], F32, "bguT"); r_bguT = Res()
        for j in range(16):
            pb = j % 2
            self.mm(ps[pb][:, 0:NE], braw[:, j * 128:(j + 1) * 128], self.ident[0:NE, 0:NE], True, True, [r_braw, self.r_ident], [ps_r[pb]], True)
            S.op("dve", (lambda e, pb=pb, j=j: e.tensor_copy(out=bguT[:, j, :], in_=ps[pb][:, 0:NE])), [ps_r[pb]], [r_bguT])
        bd = S.sb([NE, D], F32, "bd"); r_bd = Res()
        S.dma("sp", bd, self.inp["b_down"].ap()[l], [], [r_bd], "bd")
        acc = S.sb([128, NSUB, D], F32, "acc"); r_acc = [Res() for _ in range(NSUB)]
        cw = S.sb([128, NSUB, NE], F32, "cw"); r_cw = Res()
        cwT = S.sb([NE, NB], F32, "cwT"); r_cwT = Res()
        x1T = S.sb([128, NTL, 8, 512], BF16, "x1T"); r_x1T = Res()
        wgu = [S.sb([128, 8, 2 * D], BF16, "wgu") for _ in range(2)]; r_wgu = [Res(), Res()]
        wd = [S.sb([128, 8, D], BF16, "wd") for _ in range(2)]; r_wd = [Res(), Res()]
        act = [S.sb([128, 8, 512], BF16, "act") for _ in range(2)]; r_act = [Res(), Res()]
        tmp_off = S.sb_off
        gb = [S.sb([128, 512], F32, "gb") for _ in range(2)]; r_gb = [Res(), Res()]
        sg = [S.sb([128, 512], F32, "sg") for _ in range(2)]; r_sg = [Res(), Res()]
        ub = [S.sb([128, 512], F32, "ub") for _ in range(2)]; r_ub = [Res(), Res()]
        end_off = S.sb_off
        nblocks = T // NB
        nexp = NE
        if "DBG" in self.debug:
            nblocks = int(os.environ.get("DBG_MOEBLK", "1"))

        def loadw(e):
            sl = e % 2
            for c in range(0, 8, 2):
                S.dma("pool", wgu[sl][:, c:c + 2, :], wgu_d[l, e, c * 128:(c + 2) * 128, :].rearrange("(c p) n -> p c n", p=128), [], [r_wgu[sl]], f"wgu{sl}")
            for c in range(0, 8, 4):
                S.dma("pool", wd[sl][:, c:c + 4, :], wd_d[l, e, c * 128:(c + 4) * 128, :].rearrange("(c p) n -> p c n", p=128), [], [r_wd[sl]], f"wd{sl}")
        pbd = 0
        for nb in range(nblocks):
            tb = nb * NB
            loadw(0)
            S.dma("sp", cw, self.CW.ap()[tb:tb + NB, :].rearrange("(s p) e -> p s e", p=128), [self.r["CW"]], [r_cw], "mcw")
            S.dma("sp", cwT, self.CWT.ap()[:, tb:tb + NB], [self.r["CWT"]], [r_cwT], "mcwT")
            for i in range(NTL):
                S.dma("sp", x1T[:, i, :, :], self.X1T.ap()[:, tb + i * 512:tb + (i + 1) * 512].rearrange("(c p) t -> p c t", p=128), [self.r["X1T"]], [r_x1T], "mx1T")
            for s in range(NSUB):
                for hf in range(2):
                    pb = 4 + pbd % 4; pbd += 1
                    self.mm(ps[pb], cwT[:, s * 128:(s + 1) * 128], bd[:, hf * 512:(hf + 1) * 512], True, True, [r_cwT, r_bd], [ps_r[pb]], True)
                    S.op("act", (lambda e, pb=pb, s=s, hf=hf: e.copy(out=acc[:, s, hf * 512:(hf + 1) * 512], in_=ps[pb])), [ps_r[pb]], [r_acc[s]])
            steps = [(e, i) for e in range(nexp) for i in range(NTL)]
            n = len(steps)

            def GU(st):
                e, i = steps[st]
                sl = e % 2; par = st % 2
                for j in range(8):
                    jp = j % 2
                    G = 2 * jp; U = 2 * jp + 1
                    for c in range(8):
                        self.mm(ps[G], wgu[sl][:, c, j * 128:(j + 1) * 128], x1T[:, i, c, :], c == 0, c == 7, [r_wgu[sl], r_x1T], [ps_r[G]], c == 7)
                    for c in range(8):
                        self.mm(ps[U], wgu[sl][:, c, D + j * 128:D + (j + 1) * 128], x1T[:, i, c, :], c == 0, c == 7, [r_wgu[sl], r_x1T], [ps_r[U]], c == 7)
                    S.op("dve", (lambda ee, G=G, jp=jp, j=j, e=e: ee.tensor_scalar(out=gb[jp], in0=ps[G], scalar1=bguT[:, j, e:e + 1], scalar2=7.0, op0=ALU.add, op1=ALU.min)),
                         [ps_r[G], r_bguT], [r_gb[jp]])
                    S.op("act", (lambda ee, jp=jp: ee.activation(out=sg[jp], in_=gb[jp], func=AF.Sigmoid, scale=1.702)), [r_gb[jp]], [r_sg[jp]])
                    S.op("act", (lambda ee, U=U, jp=jp, j=j, e=e: ee.activation(out=ub[jp], in_=ps[U], func=AF.Identity, bias=bguT[:, 8 + j, e:e + 1], scale=1.0)),
                         [ps_r[U], r_bguT], [r_ub[jp]])
                    S.op("pool", (lambda ee, jp=jp: ee.tensor_scalar(out=ub[jp], in0=ub[jp], scalar1=-7.0, scalar2=7.0, op0=ALU.max, op1=ALU.min)), [r_ub[jp]], [r_ub[jp]])
                    S.op("pool", (lambda ee, jp=jp: ee.tensor_tensor(out=gb[jp], in0=gb[jp], in1=sg[jp], op=ALU.mult)), [r_gb[jp], r_sg[jp]], [r_gb[jp]])
                    S.op("dve", (lambda ee, jp=jp, j=j, par=par: ee.scalar_tensor_tensor(out=act[par][:, j, :], in0=ub[jp], scalar=1.0, in1=gb[jp], op0=ALU.add, op1=ALU.mult)),
                         [r_ub[jp], r_gb[jp]], [r_act[par]])

            def DOWN(st):
                nonlocal pbd
                e, i = steps[st]
                sl = e % 2; par = st % 2
                for s in range(4):
                    sub = i * 4 + s
                    for hf in range(2):
                        pb = 4 + pbd % 4; pbd += 1
                        for j in range(8):
                            self.mm(ps[pb], act[par][:, j, s * 128:(s + 1) * 128], wd[sl][:, j, hf * 512:(hf + 1) * 512], j == 0, j == 7, [r_act[par], r_wd[sl]], [ps_r[pb]], j == 7)
                        S.op("dve", (lambda ee, pb=pb, sub=sub, hf=hf, e=e: ee.scalar_tensor_tensor(out=acc[:, sub, hf * 512:(hf + 1) * 512], in0=ps[pb], scalar=cw[:, sub, e:e + 1],
                                                                                           in1=acc[:, sub, hf * 512:(hf + 1) * 512], op0=ALU.mult, op1=ALU.add)),
                             [ps_r[pb], r_cw, r_acc[sub]], [r_acc[sub]])
            for st in range(n + 1):
                if st < n:
                    GU(st)
                    if "DBG" in self.debug and st == 0 and nb == 0 and l == 0:
                        self.dump2("d_act", act[0], r_act[0], [128, 8, 512], BF16)
                        self.dump2("d_gb", gb[1], r_gb[1], [128, 512], F32)
                        self.dump2("d_ub", ub[1], r_ub[1], [128, 512], F32)
                        self.dump2("d_bguT", bguT, r_bguT, [128, 16, NE], F32)
                        self.dump2("d_acc0", acc[:, 0, :], r_acc[0], [128, D], F32)
                if st >= 1:
                    DOWN(st - 1)
                if st < n:
                    e, i = steps[st]
                    if i == 0 and e + 1 < nexp:
                        loadw(e + 1)
            if "DBG" in self.debug and nb == 0 and l == 0:
                self.dump2("d_acc1", acc[:, 0, :], r_acc[0], [128, D], F32)
            S.barrier()
            S.sb_off = tmp_off
            g2, r_g2 = self.bcast_row(self.inp["ln2_g"], l * D, D, "g2")
            b2, r_b2 = self.bcast_row(self.inp["ln2_b"], l * D, D, "b2")
            r_gb2 = Res()
            S.op("pool", lambda e: e.tensor_copy(out=g2[:, 0:1], in_=g2[:, 0:1]), [r_g2, r_b2], [r_gb2])
            lnt = self.ln_tmp()
            assert S.sb_off <= end_off + 4096
            x1t = S.sb([128, 2, D], F32, "x1t"); r_x1t = [Res(), Res()]
            for s in range(NSUB):
                q = s % 2
                S.dma("sp", x1t[:, q, :], self.X1.ap()[tb + s * 128:tb + (s + 1) * 128, :], [self.r["X1"]], [r_x1t[q]], f"mx1{q}")
                S.op("dve", (lambda e, s=s, q=q: e.scalar_tensor_tensor(out=acc[:, s, :], in0=x1t[:, q, :], scalar=ALPHA, in1=acc[:, s, :], op0=ALU.mult, op1=ALU.add)),
                     [r_x1t[q], r_acc[s]], [r_acc[s]])
                self.layer_norm(acc[:, s, :], r_acc[s], g2, b2, r_gb2, lnt)
                S.dma("sp", dst[tb + s * 128:tb + (s + 1) * 128, :], acc[:, s, :], [r_acc[s]], [r_dst], f"mout{s % 4}")
            S.barrier()
            S.sb_off = end_off


def make_in_maps(inputs, with_moe=True):
    consts = host_consts()
    maps = []
    L = DEPTH
    shared = {
        "w_in": np.ascontiguousarray(inputs["w_in"], dtype=np.float32),
        "b_gate": np.ascontiguousarray(inputs["b_gate"], dtype=np.float32),
        "diff_lambda": np.ascontiguousarray(inputs["diff_lambda"], dtype=np.float32).reshape(L, 256),
        "diff_subln_g": np.ascontiguousarray(inputs["diff_subln_g"], dtype=np.float32),
        "rel_bias": np.ascontiguousarray(inputs["rel_bias"], dtype=np.float32),
        "w_mem_kv": np.ascontiguousarray(inputs["w_mem_kv"], dtype=np.float32),
        "w_branch": np.ascontiguousarray(inputs["w_branch"], dtype=np.float32).reshape(L, 3 * W, D),
        "w_out": np.ascontiguousarray(inputs["w_out"], dtype=np.float32),
        "ln1_g": np.ascontiguousarray(inputs["ln1_g"], dtype=np.float32),
        "ln1_b": np.ascontiguousarray(inputs["ln1_b"], dtype=np.float32),
        "router_w": np.ascontiguousarray(inputs["router_w"], dtype=np.float32),
        "router_b": np.ascontiguousarray(inputs["router_b"], dtype=np.float32),
        "w_gate_up": np.ascontiguousarray(inputs["w_gate_up"], dtype=np.float32),
        "b_gate_up": np.ascontiguousarray(inputs["b_gate_up"], dtype=np.float32),
        "w_down": np.ascontiguousarray(inputs["w_down"], dtype=np.float32),
        "b_down": np.ascontiguousarray(inputs["b_down"], dtype=np.float32),
        "ln2_g": np.ascontiguousarray(inputs["ln2_g"], dtype=np.float32),
        "ln2_b": np.ascontiguousarray(inputs["ln2_b"], dtype=np.float32),
    }
    if not with_moe:
        del shared["w_gate_up"], shared["w_down"]
    for k, v in consts.items():
        shared["c_" + k] = v
    x = np.asarray(inputs["x"], dtype=np.float32)
    mem = np.asarray(inputs["mem"], dtype=np.float32)
    for c in range(NCORES):
        m = dict(shared)
        m["x"] = np.ascontiguousarray(x[c * NSEQ:(c + 1) * NSEQ].reshape(T, D))
        m["mem"] = np.ascontiguousarray(mem[c * NSEQ:(c + 1) * NSEQ].reshape(NSEQ * MEM, D))
        maps.append(m)
    return maps


def kernel(**inputs):
    b = Builder()
    maps = make_in_maps(inputs)
    res = run_bass_kernel_spmd(b.nc, maps, core_ids=list(range(NCORES)))
    outs = [np.asarray(r["out"], dtype=np.float32).reshape(NSEQ, SEQ, D) for r in res.results]
    return np.concatenate(outs, axis=0)
```

```python
import numpy as np
import concourse.bass as bass
import concourse.mybir as mybir
from concourse.bass_utils import run_bass_kernel_spmd

F32 = mybir.dt.float32
BF16 = mybir.dt.bfloat16
AF = mybir.ActivationFunctionType
ALU = mybir.AluOpType
AX = mybir.AxisListType

NCORES = 8
D = 1024
SEQ = 4096
NSEQ = 2
T = NSEQ * SEQ
DEPTH = 2
W = 512
INW = 6656
NE = 32
MEM = 256
ALPHA = (2 * DEPTH) ** 0.25
NEG = -30000.0


class Res:
    __slots__ = ("name", "w", "r", "dram", "excl")

    def __init__(self, name="", dram=False, excl=False):
        self.name = name; self.w = {}; self.r = {}; self.dram = dram; self.excl = excl


class Sched:
    ENGS = ("pe", "act", "dve", "pool", "sp")

    def __init__(self, nc):
        self.nc = nc
        self.ops = {e: [] for e in self.ENGS}
        self.sems = {}; self.cnt = {}
        self.seen = {e: {} for e in self.ENGS}
        self.pending = {e: False for e in self.ENGS}
        for e in ("pe", "act", "dve", "pool"):
            self.newsem(e)
        self.sb_off = 0
        self.sb_max = 0
        self.ARENA = 203 * 1024
        self.BASE = 20480
        self.nalloc = 0

    def newsem(self, key):
        self.sems[key] = self.nc.alloc_semaphore(f"s_{key}")
        self.cnt[key] = 0

    def sb(self, shape, dtype, name=None):
        esz = 4 if dtype == F32 else 2
        n = 1
        for s in shape[1:]:
            n *= s
        nbytes = (n * esz + 31) // 32 * 32
        off = self.sb_off
        self.sb_off += nbytes
        self.sb_max = max(self.sb_max, self.sb_off)
        assert self.sb_off <= self.ARENA, f"SBUF overflow {self.sb_off} ({name})"
        self.nalloc += 1
        t = self.nc.alloc_sbuf_tensor_at(f"{name or 't'}_{self.nalloc}", [128] + list(shape[1:]), dtype, offset=self.BASE + off)
        v = t[:]
        if shape[0] != 128:
            v = v[0:shape[0]]
        return v

    def _deps(self, eng, reads, writes):
        need = {}
        for r in reads:
            for k, v in r.w.items():
                if need.get(k, 0) < v: need[k] = v
        for w in writes:
            for k, v in w.w.items():
                if need.get(k, 0) < v: need[k] = v
            for k, v in w.r.items():
                if need.get(k, 0) < v: need[k] = v
        waits = []
        seen = self.seen[eng]
        for k, v in need.items():
            if eng == "pe" and k == "pe":
                continue
            if seen.get(k, 0) >= v:
                continue
            seen[k] = v
            waits.append((k, v))
        return waits

    def _mark(self, k, val, reads, writes):
        for r in reads:
            if r.r.get(k, 0) < val: r.r[k] = val
        for w in writes:
            if w.dram:
                if w.w.get(k, 0) < val: w.w[k] = val
            else:
                w.w = {k: val}; w.r = {}

    def op(self, eng, fn, reads=(), writes=(), inc=True):
        ex = [r for r in reads if r.excl]
        if ex:
            reads = [r for r in reads if not r.excl]
            writes = list(writes) + ex
        waits = self._deps(eng, reads, writes)
        val = self.cnt[eng] + 1
        if inc:
            self.cnt[eng] = val; self.pending[eng] = False
        else:
            self.pending[eng] = True
        self._mark(eng, val, reads, writes)
        self.ops[eng].append((waits, fn, (eng, 1) if inc else None))

    def dma(self, queue, out, in_, reads, writes, semkey, **kw):
        waits = self._deps(queue, reads, writes)
        if semkey not in self.sems:
            self.newsem(semkey)
        self.cnt[semkey] += 16
        val = self.cnt[semkey]
        self._mark(semkey, val, reads, writes)
        self.ops[queue].append((waits, (lambda e: e.dma_start(out=out, in_=in_, **kw)), (semkey, 16)))

    def barrier(self):
        for e in self.ENGS:
            assert not self.pending[e], e
            waits = []
            for k, v in self.cnt.items():
                if v == 0 or (k == e):
                    continue
                if self.seen[e].get(k, 0) >= v:
                    continue
                self.seen[e][k] = v
                waits.append((k, v))
            if waits:
                self.ops[e].append((waits, None, None))

    def new_phase(self):
        self.barrier()
        self.sb_off = self.persist_off

    def emit(self):
        self.barrier()
        nc = self.nc
        sems = self.sems
        ops = self.ops

        def run(name):
            def f(e):
                for waits, fn, inc in ops[name]:
                    for k, v in waits:
                        e.wait_ge(sems[k], v)
                    if fn is None:
                        continue
                    ins = fn(e)
                    if inc is not None:
                        ins.then_inc(sems[inc[0]], inc[1])
            return f
        with nc.Block() as block:
            block.tensor(run("pe"))
            block.scalar(run("act"))
            block.vector(run("dve"))
            block.gpsimd(run("pool"))
            block.sync(run("sp"))
        print("ops:", {k: len(v) for k, v in ops.items()}, "sems:", len(sems), "sbuf:", self.sb_max, flush=True)


def t5_bucket_np(rel):
    half = 16; max_exact = 8
    n = np.abs(rel)
    nf = np.maximum(n, 1).astype(np.float32)
    large = max_exact + (np.log(nf / max_exact) / np.log(128 / max_exact) * (half - max_exact)).astype(np.int32)
    large = np.minimum(large, half - 1)
    return np.where(rel > 0, half, 0) + np.where(n < max_exact, n, large)


def host_consts():
    c = {}
    c["ident"] = np.eye(128, dtype=np.float32)
    c["antiid"] = np.eye(128, dtype=np.float32)[::-1].copy()
    j = np.arange(128)[:, None]; s = np.arange(128)[None, :]
    c["uincl_neg"] = np.where(j >= s, -1.0, 0.0).astype(np.float32)
    c["ones_neg"] = -np.ones((128, 128), np.float32)
    c["ones"] = np.ones((128, 128), np.float32)
    c["sbmask"] = np.where(j >= s, NEG, 0.0).astype(np.float32)
    rel = np.arange(384) - 255
    b = t5_bucket_np(rel)
    oh = np.zeros((32, 384), np.float32)
    oh[b, np.arange(384)] = 1.0
    c["bucket_oh"] = oh
    return c


CONST_SHAPES = {"ident": [128, 128], "antiid": [128, 128], "uincl_neg": [128, 128], "ones_neg": [128, 128],
                "ones": [128, 128], "sbmask": [128, 128], "bucket_oh": [32, 384]}


class Builder:
    def __init__(self, n_layers=DEPTH, stop=None, debug=()):
        self.n_layers = n_layers
        self.stop = stop
        self.debug = set(debug)
        nc = self.nc = bass.Bass("TRN2", target_bir_lowering=False)
        self.S = Sched(nc)
        dt = lambda name, shape, dtype=F32, kind="ExternalInput": nc.dram_tensor(name, shape, dtype, kind=kind)
        L = DEPTH
        self.inp = {
            "x": dt("x", [T, D]), "mem": dt("mem", [NSEQ * MEM, D]),
            "w_in": dt("w_in", [L, D, INW]), "b_gate": dt("b_gate", [L, 3 * D]),
            "diff_lambda": dt("diff_lambda", [L, 256]), "diff_subln_g": dt("diff_subln_g", [L, 128]),
            "rel_bias": dt("rel_bias", [32, 8]), "w_mem_kv": dt("w_mem_kv", [L, D, D]),
            "w_branch": dt("w_branch", [L, 3 * W, D]), "w_out": dt("w_out", [L, D, D]),
            "ln1_g": dt("ln1_g", [L, D]), "ln1_b": dt("ln1_b", [L, D]),
            "router_w": dt("router_w", [L, D, NE]), "router_b": dt("router_b", [L, NE]),
            "b_gate_up": dt("b_gate_up", [L, NE, 2 * D]), "b_down": dt("b_down", [L, NE, D]),
            "ln2_g": dt("ln2_g", [L, D]), "ln2_b": dt("ln2_b", [L, D]),
        }
        import os as _os
        self.with_moe = (stop is None) or stop.startswith("p6") or bool(_os.environ.get("FORCE_MOE"))
        if self.with_moe:
            self.inp["w_gate_up"] = dt("w_gate_up", [L, NE, D, 2 * D])
            self.inp["w_down"] = dt("w_down", [L, NE, D, D])
        for k, shp in CONST_SHAPES.items():
            self.inp["c_" + k] = dt("c_" + k, shp)
        self.out = dt("out", [T, D], F32, "ExternalOutput")

        def scr(name, shape, dtype):
            kind = "ExternalOutput" if name in self.debug else "Internal"
            return nc.dram_tensor(name, shape, dtype, kind=kind)
        self.QKT = scr("QKT", [20 * 128, T], BF16)
        self.V = scr("V", [T, 1024], BF16)
        self.GT = scr("GT", [3072, T], BF16)
        self.YT = scr("YT", [1536, T], BF16)
        self.X1 = scr("X1", [T, D], F32)
        self.X1T = scr("X1T", [D, T], BF16)
        self.CW = scr("CW", [T, NE], F32)
        self.CWT = scr("CWT", [NE, T], F32)
        self.XC = scr("XC", [T, D], F32)
        self.FD = scr("FD", [8, 384], F32)
        self.WBGU = scr("WBGU", [NE * D, 2 * D], BF16)
        self.WBD = scr("WBD", [NE * D, D], BF16)
        self.r = {k: Res(k, dram=True) for k in ("QKT", "V", "GT", "YT", "X1", "X1T", "CW", "CWT", "XC", "FD", "out", "WBGU", "WBD")}
        self.ps = [nc.alloc_psum_tensor(f"ps{i}", [128, 512], F32)[:] for i in range(8)]
        self.ps_r = [Res(f"ps{i}", excl=True) for i in range(8)]
        self.build()

    def dump(self, name, ap, res, shape, dtype):
        if "DBG2" not in self.debug:
            return
        t = self.nc.dram_tensor(name, shape, dtype, kind="ExternalOutput")
        self.S.dma("sp", t.ap(), ap, [res], [Res(name, True)], "dbg_" + name)

    def mm(self, out, lhsT, rhs, start, stop, reads, writes, inc):
        self.S.op("pe", (lambda e: e.matmul(out, lhsT=lhsT, rhs=rhs, start=start, stop=stop)), reads, writes, inc)

    def load_const(self, name, dtype, queue=None):
        S = self.S
        shp = CONST_SHAPES[name]
        t = S.sb(shp, dtype, name)
        r = Res(name)
        q = "pool" if dtype == BF16 else "sp"
        S.dma(q, t, self.inp["c_" + name].ap(), [], [r], "c_" + name + ("b" if dtype == BF16 else "f"))
        return t, r

    def build(self):
        S = self.S
        self.ident, self.r_ident = self.load_const("ident", F32)
        self.ident_bf, self.r_ident_bf = self.load_const("ident", BF16)
        self.ones_bf, self.r_ones_bf = self.load_const("ones", BF16)
        self.eps_t = S.sb([128, 1], F32, "eps"); self.r_eps = Res("eps")
        S.op("pool", lambda e: e.memset(self.eps_t, 1e-5), [], [self.r_eps])
        self.Dhi = S.sb([128, 16, 128], BF16, "Dhi"); self.r_Dhi = Res("Dhi")
        self.b15 = S.sb([128, 8], F32, "b15"); self.r_b15 = Res("b15")
        S.persist_off = S.sb_off
        self.setup_bias()
        x_src = self.inp["x"].ap(); r_xsrc = Res("xin", dram=True)
        for l in range(self.n_layers):
            last = (l == self.n_layers - 1)
            if self.stop == f"pre{l}": break
            self.phase1(l, x_src, r_xsrc)
            if self.stop == f"p1_{l}": break
            self.phase_sb(l)
            if self.stop == f"p2_{l}": break
            self.phase_diff(l)
            if self.stop == f"p3_{l}": break
            self.phase_mem(l)
            if self.stop == f"p4_{l}": break
            self.phase5(l, x_src, r_xsrc)
            if self.stop == f"p5_{l}": break
            dst = self.out.ap() if last else self.XC.ap()
            r_dst = self.r["out"] if last else self.r["XC"]
            self.phase_moe(l, dst, r_dst)
            if self.stop == f"p6_{l}": break
            x_src = self.XC.ap(); r_xsrc = self.r["XC"]
        S.emit()

    def setup_bias(self):
        S = self.S; ps = self.ps; ps_r = self.ps_r
        S.new_phase()
        tab = S.sb([32, 8], F32, "tab"); r_tab = Res()
        oh = S.sb([32, 384], F32, "oh"); r_oh = Res()
        S.dma("sp", tab, self.inp["rel_bias"].ap(), [], [r_tab], "tab")
        S.dma("sp", oh, self.inp["c_bucket_oh"].ap(), [], [r_oh], "oh")
        anti, r_anti = self.load_const("antiid", F32)
        self.mm(ps[0][0:8, 0:384], tab, oh, True, True, [r_tab, r_oh], [ps_r[0]], True)
        ft = S.sb([8, 384], F32, "ft"); r_ft = Res()
        S.op("dve", lambda e: e.tensor_copy(out=ft, in_=ps[0][0:8, 0:384]), [ps_r[0]], [r_ft])
        S.dma("sp", self.FD.ap(), ft, [r_ft], [self.r["FD"]], "ft")
        S.dma("sp", self.b15, bass.AP(tensor=self.FD, offset=0, ap=[[0, 128], [384, 8]]), [self.r["FD"]], [self.r_b15],
              "b15", allow_slow_non_contiguous=True)
        xt = [S.sb([128, 128], F32, "xtoe") for _ in range(2)]; r_xt = [Res(), Res()]
        dm = [S.sb([128, 128], F32, "dm") for _ in range(2)]; r_dm = [Res(), Res()]
        k = 0
        for h in range(8):
            for d in range(2):
                base = 128 if d == 0 else 0
                b = k % 2
                S.dma("sp", xt[b], bass.AP(tensor=self.FD, offset=h * 384 + base, ap=[[1, 128], [1, 128]]),
                      [self.r["FD"]], [r_xt[b]], f"xtoe{b}")
                pb = 1 + b
                self.mm(ps[pb][:, 0:128], xt[b], anti, True, True, [r_xt[b], r_anti], [ps_r[pb]], True)
                S.op("dve", (lambda e, b=b, pb=pb, h=h: e.tensor_scalar(out=dm[b], in0=ps[pb][:, 0:128], scalar1=self.b15[:, h:h + 1],
                                                                 scalar2=None, op0=ALU.subtract)),
                     [ps_r[pb], self.r_b15], [r_dm[b]])
                if d == 0:
                    S.op("dve", (lambda e, b=b: e.memset(dm[b][64:128, 0:64], NEG)), [], [r_dm[b]])
                S.op("dve", (lambda e, b=b, h=h, d=d: e.tensor_copy(out=self.Dhi[:, d * 8 + h, :], in_=dm[b])), [r_dm[b]], [self.r_Dhi])
                k += 1

    def phase1(self, l, x_src, r_xsrc):
        S = self.S; ps = self.ps; ps_r = self.ps_r
        S.new_phase()
        w = S.sb([128, 8, INW], BF16, "w_in"); r_w = Res()
        w_in = self.inp["w_in"].ap()
        for c in range(8):
            S.dma("pool", w[:, c, :], w_in[l, c * 128:(c + 1) * 128, :], [], [r_w], "p1w")
        bg = S.sb([128, 24], F32, "bg"); r_bg = Res()
        S.dma("sp", bg, self.inp["b_gate"].ap()[l].rearrange("(j p) -> p j", p=128), [], [r_bg], "bg", allow_slow_non_contiguous=True)
        xt = [S.sb([128, 4, D], F32, "xt") for _ in range(2)]; r_xt = [Res(), Res()]
        xT = [S.sb([128, 8, 512], BF16, "xT") for _ in range(2)]; r_xT = [Res(), Res()]
        NST = 3
        stg = [S.sb([128, 4, 512], BF16, "stg") for _ in range(NST)]; r_stg = [Res() for _ in range(NST)]
        vst = [S.sb([128, 4, 1024], BF16, "vst") for _ in range(2)]; r_vst = [Res(), Res()]
        NT = T // 512
        qcols = [0, 512, 1536, 2048, 3072]
        def load(i):
            S.dma("sp", xt[i % 2], x_src[i * 512:(i + 1) * 512, :].rearrange("(s p) d -> p s d", p=128), [r_xsrc], [r_xt[i % 2]], f"p1x{i % 2}")
        load(0)
        gi = 0
        pbi = 0
        for i in range(NT):
            if i + 1 < NT:
                load(i + 1)
            b = i % 2
            t0 = i * 512
            for c in range(8):
                pb = pbi % 8; pbi += 1
                for s in range(4):
                    S.op("pe", (lambda e, pb=pb, s=s, c=c, b=b: e.transpose(out=ps[pb][:, s * 128:(s + 1) * 128], in_=xt[b][:, s, c * 128:(c + 1) * 128],
                                                                   identity=self.ident)),
                         [r_xt[b], self.r_ident], [ps_r[pb]], inc=(s == 3))
                if c % 2 == 0:
                    S.op("dve", (lambda e, pb=pb, c=c, b=b: e.tensor_copy(out=xT[b][:, c, :], in_=ps[pb])), [ps_r[pb]], [r_xT[b]])
                else:
                    S.op("act", (lambda e, pb=pb, c=c, b=b: e.copy(out=xT[b][:, c, :], in_=ps[pb])), [ps_r[pb]], [r_xT[b]])
            for g in range(11):
                sl = gi % NST; gi += 1
                for m in range(4):
                    if g < 5:
                        col = qcols[g] + m * 128
                    else:
                        col = 3584 + ((g - 5) * 4 + m) * 128
                    pb = pbi % 8; pbi += 1
                    for c in range(8):
                        self.mm(ps[pb], w[:, c, col:col + 128], xT[b][:, c, :], c == 0, c == 7, [r_w, r_xT[b]], [ps_r[pb]], c == 7)
                    if g < 5:
                        scale = 0.125 if g in (0, 2) else 1.0
                        S.op("dve", (lambda e, pb=pb, sl=sl, m=m, scale=scale: e.tensor_scalar(out=stg[sl][:, m, :], in0=ps[pb], scalar1=scale, scalar2=None,
                                                                                         op0=ALU.mult)),
                             [ps_r[pb]], [r_stg[sl]])
                    else:
                        j = (g - 5) * 4 + m
                        S.op("act", (lambda e, pb=pb, sl=sl, m=m, j=j: e.activation(out=stg[sl][:, m, :], in_=ps[pb], func=AF.Sigmoid, bias=bg[:, j:j + 1], scale=1.0)),
                             [ps_r[pb], r_bg], [r_stg[sl]])
                if g < 5:
                    dst = self.QKT.ap()[g * 512:(g + 1) * 512, t0:t0 + 512].rearrange("(c p) t -> p c t", p=128)
                    S.dma("sp", dst, stg[sl], [r_stg[sl]], [self.r["QKT"]], f"p1s{sl}")
                else:
                    dst = self.GT.ap()[(g - 5) * 512:(g - 4) * 512, t0:t0 + 512].rearrange("(c p) t -> p c t", p=128)
                    S.dma("sp", dst, stg[sl], [r_stg[sl]], [self.r["GT"]], f"p1s{sl}")
            vb = i % 2
            k = 0
            for s in range(4):
                for hv in range(2):
                    col = 1024 if hv == 0 else 2560
                    pb = pbi % 8; pbi += 1
                    for c in range(8):
                        self.mm(ps[pb], xT[b][:, c, s * 128:(s + 1) * 128], w[:, c, col:col + 512], c == 0, c == 7, [r_w, r_xT[b]], [ps_r[pb]], c == 7)
                    if k % 2 == 0:
                        S.op("dve", (lambda e, pb=pb, s=s, hv=hv, vb=vb: e.tensor_copy(out=vst[vb][:, s, hv * 512:(hv + 1) * 512], in_=ps[pb])), [ps_r[pb]], [r_vst[vb]])
                    else:
                        S.op("act", (lambda e, pb=pb, s=s, hv=hv, vb=vb: e.copy(out=vst[vb][:, s, hv * 512:(hv + 1) * 512], in_=ps[pb])), [ps_r[pb]], [r_vst[vb]])
                    k += 1
            S.dma("sp", self.V.ap()[t0:t0 + 512, :].rearrange("(s p) f -> p s f", p=128), vst[vb], [r_vst[vb]], [self.r["V"]], f"p1v{vb}")

    def phase_sb(self, l):
        S = self.S; ps = self.ps; ps_r = self.ps_r
        S.new_phase()
        uin, r_uin = self.load_const("uincl_neg", BF16)
        oneg, r_oneg = self.load_const("ones_neg", BF16)
        msk, r_msk = self.load_const("sbmask", BF16)
        idb, r_idb = self.ident_bf, self.r_ident_bf
        QT = [[S.sb([128, SEQ], BF16, "QT") for _ in range(2)] for _ in range(2)]
        r_QT = [[Res(), Res()], [Res(), Res()]]
        for hh in range(2):
            for sl_ in range(2):
                z0 = (1 - hh) * 64
                S.op("pool", (lambda e, hh=hh, sl_=sl_, z0=z0: e.memset(QT[hh][sl_][z0:z0 + 64, :], 0.0)), [], [r_QT[hh][sl_]])
        KT = [S.sb([128, SEQ], BF16, "KT") for _ in range(2)]; r_KT = [Res(), Res()]
        Vp = [S.sb([128, 32, 128], BF16, "Vp") for _ in range(2)]; r_Vp = [Res(), Res()]
        ost = [S.sb([128, SEQ], BF16, "ost") for _ in range(1)] * 2; r_ost = [Res()] * 2
        ebuf = [S.sb([128, 512], F32, "e") for _ in range(2)]; r_e = [Res(), Res()]
        sp = [S.sb([128, 512], BF16, "sp") for _ in range(2)]; r_sp = [Res(), Res()]
        aT = [S.sb([128, 512], BF16, "aT") for _ in range(2)]; r_aT = [Res(), Res()]
        lacc = S.sb([128, 512], F32, "lacc"); r_lacc = Res()
        lbf = [S.sb([128, 512], BF16, "lbf") for _ in range(3)]; r_lbf = [Res(), Res(), Res()]
        pairs = [(b, hp) for b in range(NSEQ) for hp in range(4)]
        import os
        if "DBG" in self.debug:
            pairs = pairs[:int(os.environ.get("DBG_PAIRS", "1"))]

        def load(pi):
            b, hp = pairs[pi]
            sl = pi % 2
            for hh in range(2):
                S.dma("sp", QT[hh][sl][hh * 64:(hh + 1) * 64, :], self.QKT.ap()[hp * 128 + hh * 64:hp * 128 + (hh + 1) * 64, b * SEQ:(b + 1) * SEQ],
                      [self.r["QKT"]], [r_QT[hh][sl]], f"sbq{hh}{sl}")
            S.dma("sp", KT[sl], self.QKT.ap()[512 + hp * 128:512 + (hp + 1) * 128, b * SEQ:(b + 1) * SEQ], [self.r["QKT"]], [r_KT[sl]], f"sbk{sl}")
            S.dma("sp", Vp[sl], self.V.ap()[b * SEQ:(b + 1) * SEQ, hp * 128:(hp + 1) * 128].rearrange("(k p) f -> p k f", p=128),
                  [self.r["V"]], [r_Vp[sl]], f"sbv{sl}")
        load(0)
        for pi, (b, hp) in enumerate(pairs):
            if pi + 1 < len(pairs):
                load(pi + 1)
            sl = pi % 2
            blocks = []
            for hh in range(2):
                for qb in range(8):
                    kbs = list(range(4 * qb + 3, -1, -1))
                    for idx, kb in enumerate(kbs):
                        blocks.append((hh, qb, kb, idx, idx == len(kbs) - 1))
            if "DBG" in self.debug:
                blocks = blocks[:int(os.environ.get("DBG_BLOCKS", "12"))]
            n = len(blocks)

            def zmm(bank, r_bank, hh, qb, kb, start):
                base = hh * 64
                j = kb - 4 * qb
                c0 = 128 * j if j > 0 else 0
                diag = j >= 0
                self.mm(bank[:, c0:512], KT[sl][:, kb * 128:(kb + 1) * 128], QT[hh][sl][:, qb * 512 + c0:(qb + 1) * 512],
                        start, not diag, [r_KT[sl], r_QT[hh][sl]], [r_bank], not diag)
                if diag:
                    self.mm(bank[:, c0:c0 + 128], idb, msk, False, True, [r_idb, r_msk], [r_bank], True)
                return c0

            def stageA(k):
                hh, qb, kb, idx, lastk = blocks[k]
                p = k % 2
                c0 = zmm(ps[p], ps_r[p], hh, qb, kb, True)
                S.op("act", (lambda e: e.activation(out=ebuf[p][:, c0:512], in_=ps[p][:, c0:512], func=AF.Exp)), [ps_r[p]], [r_e[p]])
                S.op("act", (lambda e: e.activation(out=sp[p][:, c0:512], in_=ebuf[p][:, c0:512], func=AF.Ln, bias=1.0, scale=1.0)), [r_e[p]], [r_sp[p]])
                if k < 6:
                    self.dump(f"d_e{k}", ebuf[p], r_e[p], [128, 512], F32)
                    self.dump(f"d_sp{k}", sp[p], r_sp[p], [128, 512], BF16)
                if not lastk:
                    if idx == 0:
                        S.op("dve", (lambda e: e.memset(lacc, 0.0)), [], [r_lacc])
                    S.op("dve", (lambda e: e.tensor_tensor(out=lacc[:, c0:512], in0=lacc[:, c0:512], in1=sp[p][:, c0:512], op=ALU.add)), [r_sp[p], r_lacc], [r_lacc])
                    q = (k + 1) % 3
                    S.op("dve", (lambda e: e.tensor_copy(out=lbf[q], in_=lacc)), [r_lacc], [r_lbf[q]])

            def stageB(k):
                hh, qb, kb, idx, lastk = blocks[k]
                p = k % 2
                bank = ps[2 + p]; r_bank = ps_r[2 + p]
                j = kb - 4 * qb
                c0 = 128 * j if j > 0 else 0
                self.mm(bank[:, c0:512], uin, sp[p][:, c0:512], True, False, [r_uin, r_sp[p]], [r_bank], False)
                if idx > 0:
                    q = k % 3
                    self.mm(bank[:, c0:512], oneg, lbf[q][:, c0:512], False, False, [r_oneg, r_lbf[q]], [r_bank], False)
                zmm(bank, r_bank, hh, qb, kb, False)
                S.op("act", (lambda e: e.activation(out=aT[p][:, c0:512], in_=bank[:, c0:512], func=AF.Exp)), [r_bank], [r_aT[p]])
                if k < 6:
                    self.dump(f"d_a{k}", aT[p], r_aT[p], [128, 512], BF16)
                    if idx > 0:
                        self.dump(f"d_l{k}", lbf[k % 3], r_lbf[k % 3], [128, 512], BF16)

            def stageC(k):
                hh, qb, kb, idx, lastk = blocks[k]
                p = k % 2
                base = hh * 64
                ob = 4 + (qb % 2)
                j = kb - 4 * qb
                c0 = 128 * j if j > 0 else 0
                self.mm(ps[ob][:, c0:512], Vp[sl][:, kb, :], aT[p][:, c0:512], idx == 0, lastk,
                        [r_Vp[sl], r_aT[p]], [ps_r[ob]], True)
                if lastk:
                    S.op("dve", (lambda e: e.tensor_copy(out=ost[sl][base:base + 64, qb * 512:(qb + 1) * 512], in_=ps[ob][base:base + 64, :])),
                         [ps_r[ob]], [r_ost[sl]])
            for step in range(n + 2):
                if step < n: stageA(step)
                if 0 <= step - 1 < n: stageB(step - 1)
                if 0 <= step - 2 < n: stageC(step - 2)
            S.dma("sp", self.YT.ap()[hp * 128:(hp + 1) * 128, b * SEQ:(b + 1) * SEQ], ost[sl], [r_ost[sl]], [self.r["YT"]], f"sbo{sl}")

    def phase_diff(self, l):
        S = self.S; ps = self.ps; ps_r = self.ps_r
        S.new_phase()
        import math, os
        lam_init = 0.8 - 0.6 * math.exp(-0.3 * l)
        idb, r_idb = self.ident_bf, self.r_ident_bf
        onesb, r_onesb = self.ones_bf, self.r_ones_bf
        dl = S.sb([128, 256], F32, "dl"); r_dl = Res()
        S.dma("sp", dl, bass.AP(tensor=self.inp["diff_lambda"], offset=l * 256, ap=[[0, 128], [1, 256]]), [], [r_dl], "dl")
        pr = S.sb([128, 2, 64], F32, "pr"); r_pr = Res()
        S.op("dve", lambda e: e.tensor_tensor(out=pr[:, 0, :], in0=dl[:, 0:64], in1=dl[:, 64:128], op=ALU.mult), [r_dl], [r_pr])
        S.op("dve", lambda e: e.tensor_tensor(out=pr[:, 1, :], in0=dl[:, 128:192], in1=dl[:, 192:256], op=ALU.mult), [r_dl], [r_pr])
        sm = S.sb([128, 4], F32, "sm"); r_sm = Res()
        S.op("dve", lambda e: e.tensor_reduce(out=sm[:, 0:2], in_=pr, axis=AX.X, op=ALU.add), [r_pr], [r_sm])
        S.op("act", lambda e: e.activation(out=sm[:, 2:4], in_=sm[:, 0:2], func=AF.Exp), [r_sm], [r_sm])
        nlam = S.sb([128, 1], F32, "nlam"); r_nlam = Res()
        S.op("dve", lambda e: e.scalar_tensor_tensor(out=nlam, in0=sm[:, 3:4], scalar=-lam_init, in1=sm[:, 2:3], op0=ALU.add, op1=ALU.subtract), [r_sm], [r_nlam])
        gp = S.sb([128, 1], F32, "gp"); r_gp = Res()
        S.dma("sp", gp, self.inp["diff_subln_g"].ap()[l].rearrange("(p o) -> p o", o=1), [], [r_gp], "gp")
        S.op("dve", lambda e: e.tensor_scalar(out=gp, in0=gp, scalar1=(1.0 - lam_init), scalar2=None, op0=ALU.mult), [r_gp], [r_gp])
        QT = [[S.sb([128, SEQ], BF16, "dQT") for _ in range(2)] for _ in range(2)]
        r_QT = [[Res(), Res()], [Res(), Res()]]
        for hh in range(2):
            for sl_ in range(2):
                z0 = (1 - hh) * 64
                S.op("pool", (lambda e, hh=hh, sl_=sl_, z0=z0: e.memset(QT[hh][sl_][z0:z0 + 64, :], 0.0)), [], [r_QT[hh][sl_]])
        KT = [S.sb([128, SEQ], BF16, "dKT") for _ in range(2)]; r_KT = [Res(), Res()]
        Vp = [S.sb([128, 32, 128], BF16, "dVp") for _ in range(2)]; r_Vp = [Res(), Res()]
        yst = S.sb([128, SEQ], BF16, "yst"); r_yst = Res()
        PT = [S.sb([128, 512], BF16, "PT") for _ in range(2)]; r_PT = [Res(), Res()]
        rr = [S.sb([128, 512], F32, "rr") for _ in range(2)]; r_rr = [Res(), Res()]
        oo = S.sb([128, 512], F32, "oo"); r_oo = Res()
        t2 = S.sb([128, 512], F32, "t2"); r_t2 = Res()
        sq = S.sb([128, 512], BF16, "sq"); r_sq = Res()
        rs = S.sb([128, 512], F32, "rs"); r_rs = Res()
        pairs = [(b, dh) for b in range(NSEQ) for dh in range(4)]
        if "DBG" in self.debug:
            pairs = pairs[:int(os.environ.get("DBG_PAIRS", "1"))]

        def load(pi):
            b, dh = pairs[pi]
            sl = pi % 2
            for hh in range(2):
                S.dma("sp", QT[hh][sl][hh * 64:(hh + 1) * 64, :], self.QKT.ap()[1024 + dh * 128 + hh * 64:1024 + dh * 128 + (hh + 1) * 64, b * SEQ:(b + 1) * SEQ],
                      [self.r["QKT"]], [r_QT[hh][sl]], f"dfq{hh}{sl}")
            S.dma("sp", KT[sl], self.QKT.ap()[1536 + dh * 128:1536 + (dh + 1) * 128, b * SEQ:(b + 1) * SEQ], [self.r["QKT"]], [r_KT[sl]], f"dfk{sl}")
            S.dma("sp", Vp[sl], self.V.ap()[b * SEQ:(b + 1) * SEQ, 512 + dh * 128:512 + (dh + 1) * 128].rearrange("(k p) f -> p k f", p=128),
                  [self.r["V"]], [r_Vp[sl]], f"dfv{sl}")
        load(0)
        kglob = 0
        for pi, (b, dh) in enumerate(pairs):
            if pi + 1 < len(pairs):
                load(pi + 1)
            sl = pi % 2
            for qb in range(8):
                for mp in range(2):
                    h = 2 * dh + mp
                    kbs = list(range(4 * qb + 3, -1, -1))
                    n = len(kbs)
                    ob = 2 + mp; db = 4 + mp

                    def stageA(idx, kg):
                        kb = kbs[idx]
                        p = kg % 2
                        j = kb - 4 * qb
                        c0 = 128 * j if j > 0 else 0
                        extra = []
                        if 0 <= j <= 3:
                            extra.append((j, 0))
                        if 0 <= j + 1 <= 3:
                            extra.append((j + 1, 1))
                        self.mm(ps[p][:, c0:512], KT[sl][:, kb * 128:(kb + 1) * 128], QT[mp][sl][:, qb * 512 + c0:(qb + 1) * 512],
                                True, len(extra) == 0, [r_KT[sl], r_QT[mp][sl]], [ps_r[p]], len(extra) == 0)
                        for ei, (qs, d) in enumerate(extra):
                            lastx = ei == len(extra) - 1
                            self.mm(ps[p][:, qs * 128:(qs + 1) * 128], idb, self.Dhi[:, d * 8 + h, :], False, lastx, [r_idb, self.r_Dhi], [ps_r[p]], lastx)
                        S.op("act", (lambda e: e.activation(out=PT[p][:, c0:512], in_=ps[p][:, c0:512], func=AF.Exp, bias=self.b15[:, h:h + 1], scale=1.0)),
                             [ps_r[p], self.r_b15], [r_PT[p]])

                    def stageB(idx, kg):
                        kb = kbs[idx]
                        p = kg % 2
                        j = kb - 4 * qb
                        c0 = 128 * j if j > 0 else 0
                        self.mm(ps[ob][:, c0:512], Vp[sl][:, kb, :], PT[p][:, c0:512], idx == 0, idx == n - 1, [r_Vp[sl], r_PT[p]], [ps_r[ob]], True)
                        self.mm(ps[db][:, c0:512], onesb, PT[p][:, c0:512], idx == 0, idx == n - 1, [r_onesb, r_PT[p]], [ps_r[db]], True)
                    for step in range(n + 1):
                        if step < n: stageA(step, kglob + step)
                        if step >= 1: stageB(step - 1, kglob + step - 1)
                    kglob += n
                    S.op("dve", (lambda e, mp=mp, db=db: e.reciprocal(out=rr[mp], in_=ps[db])), [ps_r[db]], [r_rr[mp]])
                    if mp == 0:
                        S.op("dve", (lambda e, ob=ob: e.tensor_tensor(out=oo, in0=ps[ob], in1=rr[0], op=ALU.mult)), [ps_r[ob], r_rr[0]], [r_oo])
                    else:
                        S.op("dve", (lambda e, ob=ob: e.tensor_tensor(out=t2, in0=ps[ob], in1=rr[1], op=ALU.mult)), [ps_r[ob], r_rr[1]], [r_t2])
                S.op("dve", lambda e: e.scalar_tensor_tensor(out=oo, in0=t2, scalar=nlam[:, 0:1], in1=oo, op0=ALU.mult, op1=ALU.add), [r_t2, r_nlam, r_oo], [r_oo])
                S.op("pool", lambda e: e.tensor_tensor(out=sq, in0=oo, in1=oo, op=ALU.mult), [r_oo], [r_sq])
                self.mm(ps[6], onesb, sq, True, True, [r_onesb, r_sq], [ps_r[6]], True)
                S.op("act", lambda e: e.activation(out=rs, in_=ps[6], func=AF.Sqrt, bias=self.eps_t[:, 0:1], scale=1.0 / 128.0), [ps_r[6], self.r_eps], [r_rs])
                S.op("dve", lambda e: e.reciprocal(out=rs, in_=rs), [r_rs], [r_rs])
                S.op("dve", (lambda e, qb=qb: e.scalar_tensor_tensor(out=yst[:, qb * 512:(qb + 1) * 512], in0=oo, scalar=gp[:, 0:1], in1=rs, op0=ALU.mult, op1=ALU.mult)),
                     [r_oo, r_gp, r_rs], [r_yst])
            S.dma("sp", self.YT.ap()[512 + dh * 128:512 + (dh + 1) * 128, b * SEQ:(b + 1) * SEQ], yst, [r_yst], [self.r["YT"]], "dfo")

    def phase_mem(self, l):
        S = self.S; ps = self.ps; ps_r = self.ps_r
        S.new_phase()
        onesb, r_onesb = self.ones_bf, self.r_ones_bf
        wkv = S.sb([128, 8, D], BF16, "wkv"); r_wkv = Res()
        for c in range(8):
            S.dma("pool", wkv[:, c, :], self.inp["w_mem_kv"].ap()[l, c * 128:(c + 1) * 128, :], [], [r_wkv], "wkv")
        mt = S.sb([128, 2, D], F32, "mt"); r_mt = Res()
        mT = S.sb([128, 8, MEM], BF16, "mT"); r_mT = Res()
        mKT = S.sb([128, 4, MEM], BF16, "mKT"); r_mKT = Res()
        mV = S.sb([128, 2, W], BF16, "mV"); r_mV = Res()
        QT = [S.sb([128, SEQ], BF16, "mQT") for _ in range(2)]; r_QT = [Res(), Res()]
        yst = [S.sb([128, SEQ], BF16, "myst") for _ in range(2)]; r_yst = [Res(), Res()]
        PT = [S.sb([128, 2, 512], BF16, "mPT") for _ in range(2)]; r_PT = [Res(), Res()]
        rr = S.sb([128, 512], F32, "mrr"); r_rr = Res()
        scale = 128.0 ** -0.5
        jobs = [(b, h) for b in range(NSEQ) for h in range(4)]

        def loadq(ji):
            b, h = jobs[ji]
            S.dma("sp", QT[ji % 2], self.QKT.ap()[2048 + h * 128:2048 + (h + 1) * 128, b * SEQ:(b + 1) * SEQ], [self.r["QKT"]], [r_QT[ji % 2]], f"mq{ji % 2}")
        loadq(0)
        ji = 0
        for b in range(NSEQ):
            S.dma("sp", mt, self.inp["mem"].ap()[b * MEM:(b + 1) * MEM, :].rearrange("(s p) d -> p s d", p=128), [], [r_mt], "mt")
            for c in range(8):
                pb = c % 2
                for s in range(2):
                    S.op("pe", (lambda e, pb=pb, s=s, c=c: e.transpose(out=ps[pb][:, s * 128:(s + 1) * 128], in_=mt[:, s, c * 128:(c + 1) * 128], identity=self.ident)),
                         [r_mt, self.r_ident], [ps_r[pb]], inc=(s == 1))
                S.op("dve", (lambda e, pb=pb, c=c: e.tensor_copy(out=mT[:, c, :], in_=ps[pb][:, 0:256])), [ps_r[pb]], [r_mT])
            for h in range(4):
                pb = 2 + h % 2
                for c in range(8):
                    self.mm(ps[pb][:, 0:256], wkv[:, c, h * 128:(h + 1) * 128], mT[:, c, :], c == 0, c == 7, [r_wkv, r_mT], [ps_r[pb]], c == 7)
                S.op("dve", (lambda e, pb=pb, h=h: e.tensor_copy(out=mKT[:, h, :], in_=ps[pb][:, 0:256])), [ps_r[pb]], [r_mKT])
            for mb in range(2):
                pb = 4 + mb
                for c in range(8):
                    self.mm(ps[pb], mT[:, c, mb * 128:(mb + 1) * 128], wkv[:, c, 512:1024], c == 0, c == 7, [r_wkv, r_mT], [ps_r[pb]], c == 7)
                S.op("dve", (lambda e, pb=pb, mb=mb: e.tensor_copy(out=mV[:, mb, :], in_=ps[pb])), [ps_r[pb]], [r_mV])
            for h in range(4):
                if ji + 1 < len(jobs):
                    loadq(ji + 1)
                sl = ji % 2
                for qt in range(8):
                    p = qt % 2
                    for mb in range(2):
                        zb = 0 + mb if p == 0 else 2 + mb
                        self.mm(ps[zb], mKT[:, h, mb * 128:(mb + 1) * 128], QT[sl][:, qt * 512:(qt + 1) * 512], True, True, [r_mKT, r_QT[sl]], [ps_r[zb]], True)
                        S.op("act", (lambda e, zb=zb, p=p, mb=mb: e.activation(out=PT[p][:, mb, :], in_=ps[zb], func=AF.Exp, scale=scale)), [ps_r[zb]], [r_PT[p]])
                    ob = 4 + p; db = 6 + p
                    for mb in range(2):
                        self.mm(ps[ob], mV[:, mb, h * 128:(h + 1) * 128], PT[p][:, mb, :], mb == 0, mb == 1, [r_mV, r_PT[p]], [ps_r[ob]], mb == 1)
                    for mb in range(2):
                        self.mm(ps[db], onesb, PT[p][:, mb, :], mb == 0, mb == 1, [r_onesb, r_PT[p]], [ps_r[db]], mb == 1)
                    S.op("dve", (lambda e, db=db: e.reciprocal(out=rr, in_=ps[db])), [ps_r[db]], [r_rr])
                    S.op("dve", (lambda e, ob=ob, sl=sl, qt=qt: e.tensor_tensor(out=yst[sl][:, qt * 512:(qt + 1) * 512], in0=ps[ob], in1=rr, op=ALU.mult)),
                         [ps_r[ob], r_rr], [r_yst[sl]])
                S.dma("sp", self.YT.ap()[1024 + h * 128:1024 + (h + 1) * 128, b * SEQ:(b + 1) * SEQ], yst[sl], [r_yst[sl]], [self.r["YT"]], f"mo{sl}")
                ji += 1

    def layer_norm(self, zt, r_z, g_t, b_t, r_gb, tmp):
        S = self.S
        st, mv, rstd, r_st = tmp
        for j in range(2):
            S.op("dve", (lambda e, j=j: e.bn_stats(out=st[:, j, :], in_=zt[:, j * 512:(j + 1) * 512])), [r_z], [r_st])
        S.op("dve", lambda e: e.bn_aggr(out=mv, in_=st), [r_st], [r_st])
        S.op("act", lambda e: e.activation(out=rstd, in_=mv[:, 1:2], func=AF.Sqrt, bias=self.eps_t[:, 0:1], scale=1.0), [r_st, self.r_eps], [r_st])
        S.op("dve", lambda e: e.reciprocal(out=rstd, in_=rstd), [r_st], [r_st])
        S.op("dve", lambda e: e.tensor_scalar(out=zt, in0=zt, scalar1=mv[:, 0:1], scalar2=rstd[:, 0:1], op0=ALU.subtract, op1=ALU.mult), [r_z, r_st], [r_z])
        S.op("pool", lambda e: e.tensor_tensor(out=zt, in0=zt, in1=g_t, op=ALU.mult), [r_z, r_gb], [r_z])
        S.op("pool", lambda e: e.tensor_tensor(out=zt, in0=zt, in1=b_t, op=ALU.add), [r_z, r_gb], [r_z])

    def ln_tmp(self):
        S = self.S
        return (S.sb([128, 2, 6], F32, "lnst"), S.sb([128, 2], F32, "lnmv"), S.sb([128, 1], F32, "lnrs"), Res())

    def bcast_row(self, src_tensor, offset, n, name):
        S = self.S
        t = S.sb([128, n], F32, name); r = Res()
        S.dma("sp", t, bass.AP(tensor=src_tensor, offset=offset, ap=[[0, 128], [1, n]]), [], [r], "bc_" + name)
        return t, r

    def phase5(self, l, x_src, r_xsrc):
        S = self.S; ps = self.ps; ps_r = self.ps_r
        S.new_phase()
        wb = S.sb([128, 12, D], BF16, "wb"); r_wb = Res()
        for c in range(0, 12, 4):
            S.dma("pool", wb[:, c:c + 4, :], self.inp["w_branch"].ap()[l, c * 128:(c + 4) * 128, :].rearrange("(c p) n -> p c n", p=128), [], [r_wb], "wb")
        wo = S.sb([128, 8, D], BF16, "wo"); r_wo = Res()
        for c in range(0, 8, 4):
            S.dma("pool", wo[:, c:c + 4, :], self.inp["w_out"].ap()[l, c * 128:(c + 4) * 128, :].rearrange("(c p) n -> p c n", p=128), [], [r_wo], "wo")
        rw = S.sb([128, 8, NE], F32, "rw"); r_rw = Res()
        S.dma("sp", rw, self.inp["router_w"].ap()[l].rearrange("(c p) e -> p c e", p=128), [], [r_rw], "rw")
        rb, r_rb = self.bcast_row(self.inp["router_b"], l * NE, NE, "rb")
        g1, r_g1 = self.bcast_row(self.inp["ln1_g"], l * D, D, "g1")
        b1, r_b1 = self.bcast_row(self.inp["ln1_b"], l * D, D, "b1")
        r_gb = Res()
        S.op("pool", lambda e: e.tensor_copy(out=g1[:, 0:1], in_=g1[:, 0:1]), [r_g1, r_b1], [r_gb])
        yt = [S.sb([128, 12, 512], BF16, "yt") for _ in range(2)]; r_yt = [Res(), Res()]
        gt = S.sb([128, 24, 512], BF16, "gt"); r_gt = Res()
        xt = [S.sb([128, 4, D], F32, "xt5") for _ in range(2)]; r_xt = [Res(), Res()]
        mT = S.sb([128, 8, 512], BF16, "mT"); r_mT = Res()
        mt = [S.sb([128, 512], F32, "mtmp") for _ in range(3)]; r_mt = [Res(), Res(), Res()]
        x1Tb = S.sb([128, 8, 512], BF16, "x1Tb"); r_x1Tb = Res()
        x1Tf = S.sb([128, 8, 512], F32, "x1Tf"); r_x1Tf = Res()
        lg = S.sb([128, NE], F32, "lg"); r_lg = Res()
        t8 = S.sb([128, 16], F32, "t8"); r_t8 = Res()
        ee = S.sb([128, NE], F32, "ee"); r_ee = Res()
        cw = S.sb([128, 4, NE], F32, "cw5"); r_cw = Res()
        cwT = S.sb([NE, 512], F32, "cwT5"); r_cwT = Res()
        lnt = self.ln_tmp()
        NT = T // 512
        if "DBG" in self.debug:
            import os
            NT = int(os.environ.get("DBG_TILES", "2"))

        def load(i):
            t0 = i * 512
            S.dma("sp", yt[i % 2], self.YT.ap()[:, t0:t0 + 512].rearrange("(c p) t -> p c t", p=128), [self.r["YT"]], [r_yt[i % 2]], f"p5y{i % 2}")
            S.dma("sp", xt[i % 2], x_src[t0:t0 + 512, :].rearrange("(s p) d -> p s d", p=128), [r_xsrc], [r_xt[i % 2]], f"p5x{i % 2}")
        load(0)
        pbi = 0
        for i in range(NT):
            t0 = i * 512
            b = i % 2
            S.dma("sp", gt, self.GT.ap()[:, t0:t0 + 512].rearrange("(c p) t -> p c t", p=128), [self.r["GT"]], [r_gt], "p5g")
            if i + 1 < NT:
                load(i + 1)
            for f in range(8):
                banks = []
                for br in range(3):
                    pb = pbi % 8; pbi += 1
                    banks.append(pb)
                    for k in range(4):
                        self.mm(ps[pb], wb[:, br * 4 + k, f * 128:(f + 1) * 128], yt[b][:, br * 4 + k, :], k == 0, k == 3, [r_wb, r_yt[b]], [ps_r[pb]], k == 3)
                for br in range(3):
                    pb = banks[br]
                    S.op("dve", (lambda e, pb=pb, br=br, f=f: e.tensor_tensor(out=mt[br], in0=ps[pb], in1=gt[:, br * 8 + f, :], op=ALU.mult)),
                         [ps_r[pb], r_gt], [r_mt[br]])
                S.op("pool", lambda e: e.tensor_tensor(out=mt[0], in0=mt[0], in1=mt[1], op=ALU.add), [r_mt[0], r_mt[1]], [r_mt[0]])
                S.op("pool", (lambda e, f=f: e.tensor_tensor(out=mT[:, f, :], in0=mt[0], in1=mt[2], op=ALU.add)), [r_mt[0], r_mt[2]], [r_mT])
            if "DBG" in self.debug and i == 0 and l == 0:
                self.dump2("d_mT", mT, r_mT, [128, 8, 512], BF16)
            for s in range(4):
                for hf in range(2):
                    pb = pbi % 8; pbi += 1
                    for f in range(8):
                        self.mm(ps[pb], mT[:, f, s * 128:(s + 1) * 128], wo[:, f, hf * 512:(hf + 1) * 512], f == 0, f == 7, [r_mT, r_wo], [ps_r[pb]], f == 7)
                    S.op("dve", (lambda e, pb=pb, s=s, hf=hf, b=b: e.scalar_tensor_tensor(out=xt[b][:, s, hf * 512:(hf + 1) * 512], in0=xt[b][:, s, hf * 512:(hf + 1) * 512],
                                                                                     scalar=ALPHA, in1=ps[pb], op0=ALU.mult, op1=ALU.add)),
                         [ps_r[pb], r_xt[b]], [r_xt[b]])
                self.layer_norm(xt[b][:, s, :], r_xt[b], g1, b1, r_gb, lnt)
            S.dma("sp", self.X1.ap()[t0:t0 + 512, :].rearrange("(s p) d -> p s d", p=128), xt[b], [r_xt[b]], [self.r["X1"]], f"p5o{b}")
            for c in range(8):
                pb = pbi % 8; pbi += 1
                for s in range(4):
                    S.op("pe", (lambda e, pb=pb, s=s, c=c, b=b: e.transpose(out=ps[pb][:, s * 128:(s + 1) * 128], in_=xt[b][:, s, c * 128:(c + 1) * 128],
                                                                   identity=self.ident)),
                         [r_xt[b], self.r_ident], [ps_r[pb]], inc=(s == 3))
                S.op("dve", (lambda e, pb=pb, c=c: e.tensor_copy(out=x1Tf[:, c, :], in_=ps[pb])), [ps_r[pb]], [r_x1Tf])
                S.op("act", (lambda e, c=c: e.copy(out=x1Tb[:, c, :], in_=x1Tf[:, c, :])), [r_x1Tf], [r_x1Tb])
            S.dma("sp", self.X1T.ap()[:, t0:t0 + 512].rearrange("(c p) t -> p c t", p=128), x1Tb, [r_x1Tb], [self.r["X1T"]], "p5t")
            for s in range(4):
                pb = pbi % 8; pbi += 1
                for c in range(8):
                    self.mm(ps[pb][:, 0:NE], x1Tf[:, c, s * 128:(s + 1) * 128], rw[:, c, :], c == 0, c == 7, [r_x1Tf, r_rw], [ps_r[pb]], c == 7)
                S.op("dve", (lambda e, pb=pb: e.tensor_tensor(out=lg, in0=ps[pb][:, 0:NE], in1=rb, op=ALU.add)), [ps_r[pb], r_rb], [r_lg])
                S.op("dve", lambda e: e.max(out=t8[:, 0:8], in_=lg), [r_lg], [r_t8])
                S.op("dve", lambda e: e.tensor_scalar(out=t8[:, 8:9], in0=t8[:, 0:1], scalar1=-1.0, scalar2=None, op0=ALU.mult), [r_t8], [r_t8])
                S.op("act", lambda e: e.activation(out=ee, in_=lg, func=AF.Exp, bias=t8[:, 8:9], scale=1.0), [r_lg, r_t8], [r_ee])
                S.op("dve", lambda e: e.tensor_scalar(out=lg, in0=lg, scalar1=t8[:, 3:4], scalar2=None, op0=ALU.is_ge), [r_lg, r_t8], [r_lg])
                S.op("dve", lambda e: e.tensor_tensor(out=ee, in0=ee, in1=lg, op=ALU.mult), [r_ee, r_lg], [r_ee])
                S.op("dve", lambda e: e.tensor_reduce(out=t8[:, 9:10], in_=ee, axis=AX.X, op=ALU.add), [r_ee], [r_t8])
                S.op("dve", lambda e: e.reciprocal(out=t8[:, 10:11], in_=t8[:, 9:10]), [r_t8], [r_t8])
                S.op("dve", (lambda e, s=s: e.tensor_scalar(out=cw[:, s, :], in0=ee, scalar1=t8[:, 10:11], scalar2=None, op0=ALU.mult)), [r_ee, r_t8], [r_cw])
                pb2 = pbi % 8; pbi += 1
                self.mm(ps[pb2][0:NE, 0:128], cw[:, s, :], self.ident, True, True, [r_cw, self.r_ident], [ps_r[pb2]], True)
                S.op("dve", (lambda e, pb2=pb2, s=s: e.tensor_copy(out=cwT[:, s * 128:(s + 1) * 128], in_=ps[pb2][0:NE, 0:128])), [ps_r[pb2]], [r_cwT])
            S.dma("sp", self.CW.ap()[t0:t0 + 512, :].rearrange("(s p) e -> p s e", p=128), cw, [r_cw], [self.r["CW"]], "p5c")
            S.dma("sp", self.CWT.ap()[:, t0:t0 + 512], cwT, [r_cwT], [self.r["CWT"]], "p5ct")

    def dump2(self, name, ap, res, shape, dtype):
        t = self.nc.dram_tensor(name, shape, dtype, kind="ExternalOutput")
        self.S.dma("sp", t.ap(), ap, [res], [Res(name, True)], "dbg_" + name)

    def phase_moe(self, l, dst, r_dst):
        S = self.S; ps = self.ps; ps_r = self.ps_r
        S.new_phase()
        import os
        NB = 1024
        NSUB = NB // 128
        NTL = NB // 512
        wgu_d = self.inp["w_gate_up"].ap(); wd_d = self.inp["w_down"].ap()
        braw = S.sb([NE, 2 * D], F32, "braw"); r_braw = Res()
        S.dma("sp", braw, self.inp["b_gate_up"].ap()[l], [], [r_braw], "braw")
        bguT = S.sb([128, 16, NE], F32, "bguT"); r_bguT = Res()
        for j in range(16):
            pb = j % 2
            self.mm(ps[pb][:, 0:NE], braw[:, j * 128:(j + 1) * 128], self.ident[0:NE, 0:NE], True, True, [r_braw, self.r_ident], [ps_r[pb]], True)
            S.op("dve", (lambda e, pb=pb, j=j: e.tensor_copy(out=bguT[:, j, :], in_=ps[pb][:, 0:NE])), [ps_r[pb]], [r_bguT])
        bd = S.sb([NE, D], F32, "bd"); r_bd = Res()
        S.dma("sp", bd, self.inp["b_down"].ap()[l], [], [r_bd], "bd")
        acc = S.sb([128, NSUB, D], F32, "acc"); r_acc = [Res() for _ in range(NSUB)]
        cw = S.sb([128, NSUB, NE], F32, "cw"); r_cw = Res()
        cwT = S.sb([NE, NB], F32, "cwT"); r_cwT = Res()
        x1T = S.sb([128, NTL, 8, 512], BF16, "x1T"); r_x1T = Res()
        wgu = [S.sb([128, 8, 2 * D], BF16, "wgu") for _ in range(2)]; r_wgu = [Res(), Res()]
        wd = [S.sb([128, 8, D], BF16, "wd") for _ in range(2)]; r_wd = [Res(), Res()]
        act = [S.sb([128, 8, 512], BF16, "act") for _ in range(2)]; r_act = [Res(), Res()]
        tmp_off = S.sb_off
        gb = [S.sb([128, 512], F32, "gb") for _ in range(2)]; r_gb = [Res(), Res()]
        sg = [S.sb([128, 512], F32, "sg") for _ in range(2)]; r_sg = [Res(), Res()]
        ub = [S.sb([128, 512], F32, "ub") for _ in range(2)]; r_ub = [Res(), Res()]
        end_off = S.sb_off
        nblocks = T // NB
        nexp = NE
        if "DBG" in self.debug:
            nblocks = int(os.environ.get("DBG_MOEBLK", "1"))

        def loadw(e, nb):
            sl = e % 2
            if nb == 0:
                for c in range(0, 8, 2):
                    S.dma("pool", wgu[sl][:, c:c + 2, :], wgu_d[l, e, c * 128:(c + 2) * 128, :].rearrange("(c p) n -> p c n", p=128), [], [r_wgu[sl]], f"wgu{sl}")
                for c in range(0, 8, 4):
                    S.dma("pool", wd[sl][:, c:c + 4, :], wd_d[l, e, c * 128:(c + 4) * 128, :].rearrange("(c p) n -> p c n", p=128), [], [r_wd[sl]], f"wd{sl}")
                for c in range(0, 8, 4):
                    S.dma("sp", self.WBGU.ap()[e * D + c * 128:e * D + (c + 4) * 128, :].rearrange("(c p) n -> p c n", p=128), wgu[sl][:, c:c + 4, :],
                          [r_wgu[sl]], [self.r["WBGU"]], f"wst{sl}")
                S.dma("sp", self.WBD.ap()[e * D:(e + 1) * D, :].rearrange("(c p) n -> p c n", p=128), wd[sl], [r_wd[sl]], [self.r["WBD"]], f"wst{sl}")
            else:
                for c in range(0, 8, 4):
                    S.dma("sp", wgu[sl][:, c:c + 4, :], self.WBGU.ap()[e * D + c * 128:e * D + (c + 4) * 128, :].rearrange("(c p) n -> p c n", p=128),
                          [self.r["WBGU"]], [r_wgu[sl]], f"wgu{sl}")
                S.dma("sp", wd[sl], self.WBD.ap()[e * D:(e + 1) * D, :].rearrange("(c p) n -> p c n", p=128), [self.r["WBD"]], [r_wd[sl]], f"wd{sl}")
        pbd = 0
        for nb in range(nblocks):
            tb = nb * NB
            loadw(0, nb)
            S.dma("sp", cw, self.CW.ap()[tb:tb + NB, :].rearrange("(s p) e -> p s e", p=128), [self.r["CW"]], [r_cw], "mcw")
            S.dma("sp", cwT, self.CWT.ap()[:, tb:tb + NB], [self.r["CWT"]], [r_cwT], "mcwT")
            for i in range(NTL):
                S.dma("sp", x1T[:, i, :, :], self.X1T.ap()[:, tb + i * 512:tb + (i + 1) * 512].rearrange("(c p) t -> p c t", p=128), [self.r["X1T"]], [r_x1T], "mx1T")
            for s in range(NSUB):
                for hf in range(2):
                    pb = 4 + pbd % 4; pbd += 1
                    self.mm(ps[pb], cwT[:, s * 128:(s + 1) * 128], bd[:, hf * 512:(hf + 1) * 512], True, True, [r_cwT, r_bd], [ps_r[pb]], True)
                    S.op("act", (lambda e, pb=pb, s=s, hf=hf: e.copy(out=acc[:, s, hf * 512:(hf + 1) * 512], in_=ps[pb])), [ps_r[pb]], [r_acc[s]])
            steps = [(e, i) for e in range(nexp) for i in range(NTL)]
            n = len(steps)

            def GU(st):
                e, i = steps[st]
                sl = e % 2; par = st % 2
                for j in range(8):
                    jp = j % 2
                    G = 2 * jp; U = 2 * jp + 1
                    for c in range(8):
                        self.mm(ps[G], wgu[sl][:, c, j * 128:(j + 1) * 128], x1T[:, i, c, :], c == 0, c == 7, [r_wgu[sl], r_x1T], [ps_r[G]], c == 7)
                    for c in range(8):
                        self.mm(ps[U], wgu[sl][:, c, D + j * 128:D + (j + 1) * 128], x1T[:, i, c, :], c == 0, c == 7, [r_wgu[sl], r_x1T], [ps_r[U]], c == 7)
                    S.op("dve", (lambda ee, G=G, jp=jp, j=j, e=e: ee.tensor_scalar(out=gb[jp], in0=ps[G], scalar1=bguT[:, j, e:e + 1], scalar2=7.0, op0=ALU.add, op1=ALU.min)),
                         [ps_r[G], r_bguT], [r_gb[jp]])
                    S.op("act", (lambda ee, jp=jp: ee.activation(out=sg[jp], in_=gb[jp], func=AF.Sigmoid, scale=1.702)), [r_gb[jp]], [r_sg[jp]])
                    S.op("act", (lambda ee, U=U, jp=jp, j=j, e=e: ee.activation(out=ub[jp], in_=ps[U], func=AF.Identity, bias=bguT[:, 8 + j, e:e + 1], scale=1.0)),
                         [ps_r[U], r_bguT], [r_ub[jp]])
                    S.op("pool", (lambda ee, jp=jp: ee.tensor_scalar(out=ub[jp], in0=ub[jp], scalar1=-7.0, scalar2=7.0, op0=ALU.max, op1=ALU.min)), [r_ub[jp]], [r_ub[jp]])
                    S.op("pool", (lambda ee, jp=jp: ee.tensor_tensor(out=gb[jp], in0=gb[jp], in1=sg[jp], op=ALU.mult)), [r_gb[jp], r_sg[jp]], [r_gb[jp]])
                    S.op("dve", (lambda ee, jp=jp, j=j, par=par: ee.scalar_tensor_tensor(out=act[par][:, j, :], in0=ub[jp], scalar=1.0, in1=gb[jp], op0=ALU.add, op1=ALU.mult)),
                         [r_ub[jp], r_gb[jp]], [r_act[par]])

            def DOWN(st):
                nonlocal pbd
                e, i = steps[st]
                sl = e % 2; par = st % 2
                for s in range(4):
                    sub = i * 4 + s
                    for hf in range(2):
                        pb = 4 + pbd % 4; pbd += 1
                        for j in range(8):
                            self.mm(ps[pb], act[par][:, j, s * 128:(s + 1) * 128], wd[sl][:, j, hf * 512:(hf + 1) * 512], j == 0, j == 7, [r_act[par], r_wd[sl]], [ps_r[pb]], j == 7)
                        S.op("dve", (lambda ee, pb=pb, sub=sub, hf=hf, e=e: ee.scalar_tensor_tensor(out=acc[:, sub, hf * 512:(hf + 1) * 512], in0=ps[pb], scalar=cw[:, sub, e:e + 1],
                                                                                           in1=acc[:, sub, hf * 512:(hf + 1) * 512], op0=ALU.mult, op1=ALU.add)),
                             [ps_r[pb], r_cw, r_acc[sub]], [r_acc[sub]])
            for st in range(n + 1):
                if st < n:
                    GU(st)
                    if "DBG" in self.debug and st == 0 and nb == 0 and l == 0:
                        self.dump2("d_act", act[0], r_act[0], [128, 8, 512], BF16)
                        self.dump2("d_gb", gb[1], r_gb[1], [128, 512], F32)
                        self.dump2("d_ub", ub[1], r_ub[1], [128, 512], F32)
                        self.dump2("d_bguT", bguT, r_bguT, [128, 16, NE], F32)
                        self.dump2("d_acc0", acc[:, 0, :], r_acc[0], [128, D], F32)
                if st >= 1:
                    DOWN(st - 1)
                if st < n:
                    e, i = steps[st]
                    if i == 0 and e + 1 < nexp:
                        loadw(e + 1, nb)
            if "DBG" in self.debug and nb == 0 and l == 0:
                self.dump2("d_acc1", acc[:, 0, :], r_acc[0], [128, D], F32)
            S.barrier()
            S.sb_off = tmp_off
            g2, r_g2 = self.bcast_row(self.inp["ln2_g"], l * D, D, "g2")
            b2, r_b2 = self.bcast_row(self.inp["ln2_b"], l * D, D, "b2")
            r_gb2 = Res()
            S.op("pool", lambda e: e.tensor_copy(out=g2[:, 0:1], in_=g2[:, 0:1]), [r_g2, r_b2], [r_gb2])
            lnt = self.ln_tmp()
            assert S.sb_off <= end_off + 4096
            x1t = S.sb([128, 2, D], F32, "x1t"); r_x1t = [Res(), Res()]
            for s in range(NSUB):
                q = s % 2
                S.dma("sp", x1t[:, q, :], self.X1.ap()[tb + s * 128:tb + (s + 1) * 128, :], [self.r["X1"]], [r_x1t[q]], f"mx1{q}")
                S.op("dve", (lambda e, s=s, q=q: e.scalar_tensor_tensor(out=acc[:, s, :], in0=x1t[:, q, :], scalar=ALPHA, in1=acc[:, s, :], op0=ALU.mult, op1=ALU.add)),
                     [r_x1t[q], r_acc[s]], [r_acc[s]])
                self.layer_norm(acc[:, s, :], r_acc[s], g2, b2, r_gb2, lnt)
                S.dma("sp", dst[tb + s * 128:tb + (s + 1) * 128, :], acc[:, s, :], [r_acc[s]], [r_dst], f"mout{s % 4}")
            S.barrier()
            S.sb_off = end_off


def make_in_maps(inputs, with_moe=True):
    consts = host_consts()
    maps = []
    L = DEPTH
    shared = {
        "w_in": np.ascontiguousarray(inputs["w_in"], dtype=np.float32),
        "b_gate": np.ascontiguousarray(inputs["b_gate"], dtype=np.float32),
        "diff_lambda": np.ascontiguousarray(inputs["diff_lambda"], dtype=np.float32).reshape(L, 256),
        "diff_subln_g": np.ascontiguousarray(inputs["diff_subln_g"], dtype=np.float32),
        "rel_bias": np.ascontiguousarray(inputs["rel_bias"], dtype=np.float32),
        "w_mem_kv": np.ascontiguousarray(inputs["w_mem_kv"], dtype=np.float32),
        "w_branch": np.ascontiguousarray(inputs["w_branch"], dtype=np.float32).reshape(L, 3 * W, D),
        "w_out": np.ascontiguousarray(inputs["w_out"], dtype=np.float32),
        "ln1_g": np.ascontiguousarray(inputs["ln1_g"], dtype=np.float32),
        "ln1_b": np.ascontiguousarray(inputs["ln1_b"], dtype=np.float32),
        "router_w": np.ascontiguousarray(inputs["router_w"], dtype=np.float32),
        "router_b": np.ascontiguousarray(inputs["router_b"], dtype=np.float32),
        "w_gate_up": np.ascontiguousarray(inputs["w_gate_up"], dtype=np.float32),
        "b_gate_up": np.ascontiguousarray(inputs["b_gate_up"], dtype=np.float32),
        "w_down": np.ascontiguousarray(inputs["w_down"], dtype=np.float32),
        "b_down": np.ascontiguousarray(inputs["b_down"], dtype=np.float32),
        "ln2_g": np.ascontiguousarray(inputs["ln2_g"], dtype=np.float32),
        "ln2_b": np.ascontiguousarray(inputs["ln2_b"], dtype=np.float32),
    }
    if not with_moe:
        del shared["w_gate_up"], shared["w_down"]
    for k, v in consts.items():
        shared["c_" + k] = v
    x = np.asarray(inputs["x"], dtype=np.float32)
    mem = np.asarray(inputs["mem"], dtype=np.float32)
    for c in range(NCORES):
        m = dict(shared)
        m["x"] = np.ascontiguousarray(x[c * NSEQ:(c + 1) * NSEQ].reshape(T, D))
        m["mem"] = np.ascontiguousarray(mem[c * NSEQ:(c + 1) * NSEQ].reshape(NSEQ * MEM, D))
        maps.append(m)
    return maps


def kernel(**inputs):
    b = Builder()
    maps = make_in_maps(inputs)
    res = run_bass_kernel_spmd(b.nc, maps, core_ids=list(range(NCORES)))
    outs = [np.asarray(r["out"], dtype=np.float32).reshape(NSEQ, SEQ, D) for r in res.results]
    return np.concatenate(outs, axis=0)
```

```python
import numpy as np
import concourse.bass as bass
import concourse.mybir as mybir
from concourse.bass_utils import run_bass_kernel_spmd

F32 = mybir.dt.float32
BF16 = mybir.dt.bfloat16
AF = mybir.ActivationFunctionType
ALU = mybir.AluOpType
AX = mybir.AxisListType

NCORES = 8
D = 1024
SEQ = 4096
NSEQ = 2
T = NSEQ * SEQ
DEPTH = 2
W = 512
INW = 6656
NE = 32
MEM = 256
ALPHA = (2 * DEPTH) ** 0.25
NEG = -30000.0


class Res:
    __slots__ = ("name", "w", "r", "dram", "excl")

    def __init__(self, name="", dram=False, excl=False):
        self.name = name; self.w = {}; self.r = {}; self.dram = dram; self.excl = excl


class Sched:
    ENGS = ("pe", "act", "dve", "pool", "sp")

    def __init__(self, nc):
        self.nc = nc
        self.ops = {e: [] for e in self.ENGS}
        self.sems = {}; self.cnt = {}
        self.seen = {e: {} for e in self.ENGS}
        self.pending = {e: False for e in self.ENGS}
        for e in ("pe", "act", "dve", "pool"):
            self.newsem(e)
        self.sb_off = 0
        self.sb_max = 0
        self.ARENA = 203 * 1024
        self.BASE = 20480
        self.nalloc = 0

    def newsem(self, key):
        self.sems[key] = self.nc.alloc_semaphore(f"s_{key}")
        self.cnt[key] = 0

    def sb(self, shape, dtype, name=None):
        esz = 4 if dtype == F32 else 2
        n = 1
        for s in shape[1:]:
            n *= s
        nbytes = (n * esz + 31) // 32 * 32
        off = self.sb_off
        self.sb_off += nbytes
        self.sb_max = max(self.sb_max, self.sb_off)
        assert self.sb_off <= self.ARENA, f"SBUF overflow {self.sb_off} ({name})"
        self.nalloc += 1
        t = self.nc.alloc_sbuf_tensor_at(f"{name or 't'}_{self.nalloc}", [128] + list(shape[1:]), dtype, offset=self.BASE + off)
        v = t[:]
        if shape[0] != 128:
            v = v[0:shape[0]]
        return v

    def _deps(self, eng, reads, writes):
        need = {}
        for r in reads:
            for k, v in r.w.items():
                if need.get(k, 0) < v: need[k] = v
        for w in writes:
            for k, v in w.w.items():
                if need.get(k, 0) < v: need[k] = v
            for k, v in w.r.items():
                if need.get(k, 0) < v: need[k] = v
        waits = []
        seen = self.seen[eng]
        for k, v in need.items():
            if eng == "pe" and k == "pe":
                continue
            if seen.get(k, 0) >= v:
                continue
            seen[k] = v
            waits.append((k, v))
        return waits

    def _mark(self, k, val, reads, writes):
        for r in reads:
            if r.r.get(k, 0) < val: r.r[k] = val
        for w in writes:
            if w.dram:
                if w.w.get(k, 0) < val: w.w[k] = val
            else:
                w.w = {k: val}; w.r = {}

    def op(self, eng, fn, reads=(), writes=(), inc=True):
        ex = [r for r in reads if r.excl]
        if ex:
            reads = [r for r in reads if not r.excl]
            writes = list(writes) + ex
        waits = self._deps(eng, reads, writes)
        val = self.cnt[eng] + 1
        if inc:
            self.cnt[eng] = val; self.pending[eng] = False
        else:
            self.pending[eng] = True
        self._mark(eng, val, reads, writes)
        self.ops[eng].append((waits, fn, (eng, 1) if inc else None))

    def dma(self, queue, out, in_, reads, writes, semkey, **kw):
        waits = self._deps(queue, reads, writes)
        if semkey not in self.sems:
            self.newsem(semkey)
        self.cnt[semkey] += 16
        val = self.cnt[semkey]
        self._mark(semkey, val, reads, writes)
        self.ops[queue].append((waits, (lambda e: e.dma_start(out=out, in_=in_, **kw)), (semkey, 16)))

    def barrier(self):
        for e in self.ENGS:
            assert not self.pending[e], e
            waits = []
            for k, v in self.cnt.items():
                if v == 0 or (k == e):
                    continue
                if self.seen[e].get(k, 0) >= v:
                    continue
                self.seen[e][k] = v
                waits.append((k, v))
            if waits:
                self.ops[e].append((waits, None, None))

    def new_phase(self):
        self.barrier()
        self.sb_off = self.persist_off

    def emit(self):
        self.barrier()
        nc = self.nc
        sems = self.sems
        ops = self.ops

        def run(name):
            def f(e):
                for waits, fn, inc in ops[name]:
                    for k, v in waits:
                        e.wait_ge(sems[k], v)
                    if fn is None:
                        continue
                    ins = fn(e)
                    if inc is not None:
                        ins.then_inc(sems[inc[0]], inc[1])
            return f
        with nc.Block() as block:
            block.tensor(run("pe"))
            block.scalar(run("act"))
            block.vector(run("dve"))
            block.gpsimd(run("pool"))
            block.sync(run("sp"))
        print("ops:", {k: len(v) for k, v in ops.items()}, "sems:", len(sems), "sbuf:", self.sb_max, flush=True)


def t5_bucket_np(rel):
    half = 16; max_exact = 8
    n = np.abs(rel)
    nf = np.maximum(n, 1).astype(np.float32)
    large = max_exact + (np.log(nf / max_exact) / np.log(128 / max_exact) * (half - max_exact)).astype(np.int32)
    large = np.minimum(large, half - 1)
    return np.where(rel > 0, half, 0) + np.where(n < max_exact, n, large)


def host_consts():
    c = {}
    c["ident"] = np.eye(128, dtype=np.float32)
    c["antiid"] = np.eye(128, dtype=np.float32)[::-1].copy()
    j = np.arange(128)[:, None]; s = np.arange(128)[None, :]
    c["uincl_neg"] = np.where(j >= s, -1.0, 0.0).astype(np.float32)
    c["ones_neg"] = -np.ones((128, 128), np.float32)
    c["ones"] = np.ones((128, 128), np.float32)
    c["sbmask"] = np.where(j >= s, NEG, 0.0).astype(np.float32)
    rel = np.arange(384) - 255
    b = t5_bucket_np(rel)
    oh = np.zeros((32, 384), np.float32)
    oh[b, np.arange(384)] = 1.0
    c["bucket_oh"] = oh
    return c


CONST_SHAPES = {"ident": [128, 128], "antiid": [128, 128], "uincl_neg": [128, 128], "ones_neg": [128, 128],
                "ones": [128, 128], "sbmask": [128, 128], "bucket_oh": [32, 384]}


class Builder:
    def __init__(self, n_layers=DEPTH, stop=None, debug=()):
        self.n_layers = n_layers
        self.stop = stop
        self.debug = set(debug)
        nc = self.nc = bass.Bass("TRN2", target_bir_lowering=False)
        self.S = Sched(nc)
        dt = lambda name, shape, dtype=F32, kind="ExternalInput": nc.dram_tensor(name, shape, dtype, kind=kind)
        L = DEPTH
        self.inp = {
            "x": dt("x", [T, D]), "mem": dt("mem", [NSEQ * MEM, D]),
            "w_in": dt("w_in", [L, D, INW]), "b_gate": dt("b_gate", [L, 3 * D]),
            "diff_lambda": dt("diff_lambda", [L, 256]), "diff_subln_g": dt("diff_subln_g", [L, 128]),
            "rel_bias": dt("rel_bias", [32, 8]), "w_mem_kv": dt("w_mem_kv", [L, D, D]),
            "w_branch": dt("w_branch", [L, 3 * W, D]), "w_out": dt("w_out", [L, D, D]),
            "ln1_g": dt("ln1_g", [L, D]), "ln1_b": dt("ln1_b", [L, D]),
            "router_w": dt("router_w", [L, D, NE]), "router_b": dt("router_b", [L, NE]),
            "b_gate_up": dt("b_gate_up", [L, NE, 2 * D]), "b_down": dt("b_down", [L, NE, D]),
            "ln2_g": dt("ln2_g", [L, D]), "ln2_b": dt("ln2_b", [L, D]),
        }
        import os as _os
        self.with_moe = (stop is None) or stop.startswith("p6") or bool(_os.environ.get("FORCE_MOE"))
        if self.with_moe:
            self.inp["w_gate_up"] = dt("w_gate_up", [L, NE, D, 2 * D])
            self.inp["w_down"] = dt("w_down", [L, NE, D, D])
        for k, shp in CONST_SHAPES.items():
            self.inp["c_" + k] = dt("c_" + k, shp)
        self.out = dt("out", [T, D], F32, "ExternalOutput")

        def scr(name, shape, dtype):
            kind = "ExternalOutput" if name in self.debug else "Internal"
            return nc.dram_tensor(name, shape, dtype, kind=kind)
        self.QKT = scr("QKT", [20 * 128, T], BF16)
        self.V = scr("V", [T, 1024], BF16)
        self.GT = scr("GT", [3072, T], BF16)
        self.YT = scr("YT", [1536, T], BF16)
        self.X1 = scr("X1", [T, D], F32)
        self.X1T = scr("X1T", [D, T], BF16)
        self.CW = scr("CW", [T, NE], F32)
        self.CWT = scr("CWT", [NE, T], F32)
        self.XC = scr("XC", [T, D], F32)
        self.FD = scr("FD", [8, 384], F32)
        self.WBGU = scr("WBGU", [NE * D, 2 * D], BF16)
        self.WBD = scr("WBD", [NE * D, D], BF16)
        self.r = {k: Res(k, dram=True) for k in ("QKT", "V", "GT", "YT", "X1", "X1T", "CW", "CWT", "XC", "FD", "out", "WBGU", "WBD")}
        self.ps = [nc.alloc_psum_tensor(f"ps{i}", [128, 512], F32)[:] for i in range(8)]
        self.ps_r = [Res(f"ps{i}", excl=True) for i in range(8)]
        self.build()

    def dump(self, name, ap, res, shape, dtype):
        if "DBG2" not in self.debug:
            return
        t = self.nc.dram_tensor(name, shape, dtype, kind="ExternalOutput")
        self.S.dma("sp", t.ap(), ap, [res], [Res(name, True)], "dbg_" + name)

    def mm(self, out, lhsT, rhs, start, stop, reads, writes, inc):
        self.S.op("pe", (lambda e: e.matmul(out, lhsT=lhsT, rhs=rhs, start=start, stop=stop)), reads, writes, inc)

    def load_const(self, name, dtype, queue=None):
        S = self.S
        shp = CONST_SHAPES[name]
        t = S.sb(shp, dtype, name)
        r = Res(name)
        q = "pool" if dtype == BF16 else "sp"
        S.dma(q, t, self.inp["c_" + name].ap(), [], [r], "c_" + name + ("b" if dtype == BF16 else "f"))
        return t, r

    def build(self):
        S = self.S
        self.ident, self.r_ident = self.load_const("ident", F32)
        self.ident_bf, self.r_ident_bf = self.load_const("ident", BF16)
        self.ones_bf, self.r_ones_bf = self.load_const("ones", BF16)
        self.eps_t = S.sb([128, 1], F32, "eps"); self.r_eps = Res("eps")
        S.op("pool", lambda e: e.memset(self.eps_t, 1e-5), [], [self.r_eps])
        self.Dhi = S.sb([128, 16, 128], BF16, "Dhi"); self.r_Dhi = Res("Dhi")
        self.b15 = S.sb([128, 8], F32, "b15"); self.r_b15 = Res("b15")
        S.persist_off = S.sb_off
        self.setup_bias()
        x_src = self.inp["x"].ap(); r_xsrc = Res("xin", dram=True)
        for l in range(self.n_layers):
            last = (l == self.n_layers - 1)
            if self.stop == f"pre{l}": break
            self.phase1(l, x_src, r_xsrc)
            if self.stop == f"p1_{l}": break
            self.phase_sb(l)
            if self.stop == f"p2_{l}": break
            self.phase_diff(l)
            if self.stop == f"p3_{l}": break
            self.phase_mem(l)
            if self.stop == f"p4_{l}": break
            self.phase5(l, x_src, r_xsrc)
            if self.stop == f"p5_{l}": break
            dst = self.out.ap() if last else self.XC.ap()
            r_dst = self.r["out"] if last else self.r["XC"]
            self.phase_moe(l, dst, r_dst)
            if self.stop == f"p6_{l}": break
            x_src = self.XC.ap(); r_xsrc = self.r["XC"]
        S.emit()

    def setup_bias(self):
        S = self.S; ps = self.ps; ps_r = self.ps_r
        S.new_phase()
        tab = S.sb([32, 8], F32, "tab"); r_tab = Res()
        oh = S.sb([32, 384], F32, "oh"); r_oh = Res()
        S.dma("sp", tab, self.inp["rel_bias"].ap(), [], [r_tab], "tab")
        S.dma("sp", oh, self.inp["c_bucket_oh"].ap(), [], [r_oh], "oh")
        anti, r_anti = self.load_const("antiid", F32)
        self.mm(ps[0][0:8, 0:384], tab, oh, True, True, [r_tab, r_oh], [ps_r[0]], True)
        ft = S.sb([8, 384], F32, "ft"); r_ft = Res()
        S.op("dve", lambda e: e.tensor_copy(out=ft, in_=ps[0][0:8, 0:384]), [ps_r[0]], [r_ft])
        S.dma("sp", self.FD.ap(), ft, [r_ft], [self.r["FD"]], "ft")
        S.dma("sp", self.b15, bass.AP(tensor=self.FD, offset=0, ap=[[0, 128], [384, 8]]), [self.r["FD"]], [self.r_b15],
              "b15", allow_slow_non_contiguous=True)
        xt = [S.sb([128, 128], F32, "xtoe") for _ in range(2)]; r_xt = [Res(), Res()]
        dm = [S.sb([128, 128], F32, "dm") for _ in range(2)]; r_dm = [Res(), Res()]
        k = 0
        for h in range(8):
            for d in range(2):
                base = 128 if d == 0 else 0
                b = k % 2
                S.dma("sp", xt[b], bass.AP(tensor=self.FD, offset=h * 384 + base, ap=[[1, 128], [1, 128]]),
                      [self.r["FD"]], [r_xt[b]], f"xtoe{b}")
                pb = 1 + b
                self.mm(ps[pb][:, 0:128], xt[b], anti, True, True, [r_xt[b], r_anti], [ps_r[pb]], True)
                S.op("dve", (lambda e, b=b, pb=pb, h=h: e.tensor_scalar(out=dm[b], in0=ps[pb][:, 0:128], scalar1=self.b15[:, h:h + 1],
                                                                 scalar2=None, op0=ALU.subtract)),
                     [ps_r[pb], self.r_b15], [r_dm[b]])
                if d == 0:
                    S.op("dve", (lambda e, b=b: e.memset(dm[b][64:128, 0:64], NEG)), [], [r_dm[b]])
                S.op("dve", (lambda e, b=b, h=h, d=d: e.tensor_copy(out=self.Dhi[:, d * 8 + h, :], in_=dm[b])), [r_dm[b]], [self.r_Dhi])
                k += 1

    def phase1(self, l, x_src, r_xsrc):
        S = self.S; ps = self.ps; ps_r = self.ps_r
        S.new_phase()
        w = S.sb([128, 8, INW], BF16, "w_in"); r_w = Res()
        w_in = self.inp["w_in"].ap()
        for c in range(8):
            S.dma("pool", w[:, c, :], w_in[l, c * 128:(c + 1) * 128, :], [], [r_w], "p1w")
        bg = S.sb([128, 24], F32, "bg"); r_bg = Res()
        S.dma("sp", bg, self.inp["b_gate"].ap()[l].rearrange("(j p) -> p j", p=128), [], [r_bg], "bg", allow_slow_non_contiguous=True)
        xt = [S.sb([128, 4, D], F32, "xt") for _ in range(2)]; r_xt = [Res(), Res()]
        xT = [S.sb([128, 8, 512], BF16, "xT") for _ in range(2)]; r_xT = [Res(), Res()]
        NST = 3
        stg = [S.sb([128, 4, 512], BF16, "stg") for _ in range(NST)]; r_stg = [Res() for _ in range(NST)]
        vst = [S.sb([128, 4, 1024], BF16, "vst") for _ in range(2)]; r_vst = [Res(), Res()]
        NT = T // 512
        qcols = [0, 512, 1536, 2048, 3072]
        def load(i):
            S.dma("sp", xt[i % 2], x_src[i * 512:(i + 1) * 512, :].rearrange("(s p) d -> p s d", p=128), [r_xsrc], [r_xt[i % 2]], f"p1x{i % 2}")
        load(0)
        gi = 0
        pbi = 0
        for i in range(NT):
            if i + 1 < NT:
                load(i + 1)
            b = i % 2
            t0 = i * 512
            for c in range(8):
                pb = pbi % 8; pbi += 1
                for s in range(4):
                    S.op("pe", (lambda e, pb=pb, s=s, c=c, b=b: e.transpose(out=ps[pb][:, s * 128:(s + 1) * 128], in_=xt[b][:, s, c * 128:(c + 1) * 128],
                                                                   identity=self.ident)),
                         [r_xt[b], self.r_ident], [ps_r[pb]], inc=(s == 3))
                if c % 2 == 0:
                    S.op("dve", (lambda e, pb=pb, c=c, b=b: e.tensor_copy(out=xT[b][:, c, :], in_=ps[pb])), [ps_r[pb]], [r_xT[b]])
                else:
                    S.op("act", (lambda e, pb=pb, c=c, b=b: e.copy(out=xT[b][:, c, :], in_=ps[pb])), [ps_r[pb]], [r_xT[b]])
            for g in range(11):
                sl = gi % NST; gi += 1
                for m in range(4):
                    if g < 5:
                        col = qcols[g] + m * 128
                    else:
                        col = 3584 + ((g - 5) * 4 + m) * 128
                    pb = pbi % 8; pbi += 1
                    for c in range(8):
                        self.mm(ps[pb], w[:, c, col:col + 128], xT[b][:, c, :], c == 0, c == 7, [r_w, r_xT[b]], [ps_r[pb]], c == 7)
                    if g < 5:
                        scale = 0.125 if g in (0, 2) else 1.0
                        S.op("dve", (lambda e, pb=pb, sl=sl, m=m, scale=scale: e.tensor_scalar(out=stg[sl][:, m, :], in0=ps[pb], scalar1=scale, scalar2=None,
                                                                                         op0=ALU.mult)),
                             [ps_r[pb]], [r_stg[sl]])
                    else:
                        j = (g - 5) * 4 + m
                        S.op("act", (lambda e, pb=pb, sl=sl, m=m, j=j: e.activation(out=stg[sl][:, m, :], in_=ps[pb], func=AF.Sigmoid, bias=bg[:, j:j + 1], scale=1.0)),
                             [ps_r[pb], r_bg], [r_stg[sl]])
                if g < 5:
                    dst = self.QKT.ap()[g * 512:(g + 1) * 512, t0:t0 + 512].rearrange("(c p) t -> p c t", p=128)
                    S.dma("sp", dst, stg[sl], [r_stg[sl]], [self.r["QKT"]], f"p1s{sl}")
                else:
                    dst = self.GT.ap()[(g - 5) * 512:(g - 4) * 512, t0:t0 + 512].rearrange("(c p) t -> p c t", p=128)
                    S.dma("sp", dst, stg[sl], [r_stg[sl]], [self.r["GT"]], f"p1s{sl}")
            vb = i % 2
            k = 0
            for s in range(4):
                for hv in range(2):
                    col = 1024 if hv == 0 else 2560
                    pb = pbi % 8; pbi += 1
                    for c in range(8):
                        self.mm(ps[pb], xT[b][:, c, s * 128:(s + 1) * 128], w[:, c, col:col + 512], c == 0, c == 7, [r_w, r_xT[b]], [ps_r[pb]], c == 7)
                    if k % 2 == 0:
                        S.op("dve", (lambda e, pb=pb, s=s, hv=hv, vb=vb: e.tensor_copy(out=vst[vb][:, s, hv * 512:(hv + 1) * 512], in_=ps[pb])), [ps_r[pb]], [r_vst[vb]])
                    else:
                        S.op("act", (lambda e, pb=pb, s=s, hv=hv, vb=vb: e.copy(out=vst[vb][:, s, hv * 512:(hv + 1) * 512], in_=ps[pb])), [ps_r[pb]], [r_vst[vb]])
                    k += 1
            S.dma("sp", self.V.ap()[t0:t0 + 512, :].rearrange("(s p) f -> p s f", p=128), vst[vb], [r_vst[vb]], [self.r["V"]], f"p1v{vb}")

    def phase_sb(self, l):
        S = self.S; ps = self.ps; ps_r = self.ps_r
        S.new_phase()
        uin, r_uin = self.load_const("uincl_neg", BF16)
        oneg, r_oneg = self.load_const("ones_neg", BF16)
        msk, r_msk = self.load_const("sbmask", BF16)
        idb, r_idb = self.ident_bf, self.r_ident_bf
        QT = [[S.sb([128, SEQ], BF16, "QT") for _ in range(2)] for _ in range(2)]
        r_QT = [[Res(), Res()], [Res(), Res()]]
        for hh in range(2):
            for sl_ in range(2):
                z0 = (1 - hh) * 64
                S.op("pool", (lambda e, hh=hh, sl_=sl_, z0=z0: e.memset(QT[hh][sl_][z0:z0 + 64, :], 0.0)), [], [r_QT[hh][sl_]])
        KT = [S.sb([128, SEQ], BF16, "KT") for _ in range(2)]; r_KT = [Res(), Res()]
        Vp = [S.sb([128, 32, 128], BF16, "Vp") for _ in range(2)]; r_Vp = [Res(), Res()]
        ost = [S.sb([128, SEQ], BF16, "ost") for _ in range(1)] * 2; r_ost = [Res()] * 2
        ebuf = [S.sb([128, 512], F32, "e") for _ in range(2)]; r_e = [Res(), Res()]
        sp = [S.sb([128, 512], BF16, "sp") for _ in range(2)]; r_sp = [Res(), Res()]
        aT = [S.sb([128, 512], BF16, "aT") for _ in range(2)]; r_aT = [Res(), Res()]
        lacc = S.sb([128, 512], F32, "lacc"); r_lacc = Res()
        lbf = [S.sb([128, 512], BF16, "lbf") for _ in range(3)]; r_lbf = [Res(), Res(), Res()]
        pairs = [(b, hp) for b in range(NSEQ) for hp in range(4)]
        import os
        if "DBG" in self.debug:
            pairs = pairs[:int(os.environ.get("DBG_PAIRS", "1"))]

        def load(pi):
            b, hp = pairs[pi]
            sl = pi % 2
            for hh in range(2):
                S.dma("sp", QT[hh][sl][hh * 64:(hh + 1) * 64, :], self.QKT.ap()[hp * 128 + hh * 64:hp * 128 + (hh + 1) * 64, b * SEQ:(b + 1) * SEQ],
                      [self.r["QKT"]], [r_QT[hh][sl]], f"sbq{hh}{sl}")
            S.dma("sp", KT[sl], self.QKT.ap()[512 + hp * 128:512 + (hp + 1) * 128, b * SEQ:(b + 1) * SEQ], [self.r["QKT"]], [r_KT[sl]], f"sbk{sl}")
            S.dma("sp", Vp[sl], self.V.ap()[b * SEQ:(b + 1) * SEQ, hp * 128:(hp + 1) * 128].rearrange("(k p) f -> p k f", p=128),
                  [self.r["V"]], [r_Vp[sl]], f"sbv{sl}")
        load(0)
        for pi, (b, hp) in enumerate(pairs):
            if pi + 1 < len(pairs):
                load(pi + 1)
            sl = pi % 2
            blocks = []
            for hh in range(2):
                for qb in range(8):
                    kbs = list(range(4 * qb + 3, -1, -1))
                    for idx, kb in enumerate(kbs):
                        blocks.append((hh, qb, kb, idx, idx == len(kbs) - 1))
            if "DBG" in self.debug:
                blocks = blocks[:int(os.environ.get("DBG_BLOCKS", "12"))]
            n = len(blocks)

            def zmm(bank, r_bank, hh, qb, kb, start):
                base = hh * 64
                j = kb - 4 * qb
                c0 = 128 * j if j > 0 else 0
                diag = j >= 0
                self.mm(bank[:, c0:512], KT[sl][:, kb * 128:(kb + 1) * 128], QT[hh][sl][:, qb * 512 + c0:(qb + 1) * 512],
                        start, not diag, [r_KT[sl], r_QT[hh][sl]], [r_bank], not diag)
                if diag:
                    self.mm(bank[:, c0:c0 + 128], idb, msk, False, True, [r_idb, r_msk], [r_bank], True)
                return c0

            def stageA(k):
                hh, qb, kb, idx, lastk = blocks[k]
                p = k % 2
                c0 = zmm(ps[p], ps_r[p], hh, qb, kb, True)
                S.op("act", (lambda e: e.activation(out=ebuf[p][:, c0:512], in_=ps[p][:, c0:512], func=AF.Exp)), [ps_r[p]], [r_e[p]])
                S.op("act", (lambda e: e.activation(out=sp[p][:, c0:512], in_=ebuf[p][:, c0:512], func=AF.Ln, bias=1.0, scale=1.0)), [r_e[p]], [r_sp[p]])
                if k < 6:
                    self.dump(f"d_e{k}", ebuf[p], r_e[p], [128, 512], F32)
                    self.dump(f"d_sp{k}", sp[p], r_sp[p], [128, 512], BF16)
                if not lastk:
                    if idx == 0:
                        S.op("dve", (lambda e: e.memset(lacc, 0.0)), [], [r_lacc])
                    S.op("dve", (lambda e: e.tensor_tensor(out=lacc[:, c0:512], in0=lacc[:, c0:512], in1=sp[p][:, c0:512], op=ALU.add)), [r_sp[p], r_lacc], [r_lacc])
                    q = (k + 1) % 3
                    S.op("dve", (lambda e: e.tensor_copy(out=lbf[q], in_=lacc)), [r_lacc], [r_lbf[q]])

            def stageB(k):
                hh, qb, kb, idx, lastk = blocks[k]
                p = k % 2
                bank = ps[2 + p]; r_bank = ps_r[2 + p]
                j = kb - 4 * qb
                c0 = 128 * j if j > 0 else 0
                self.mm(bank[:, c0:512], uin, sp[p][:, c0:512], True, False, [r_uin, r_sp[p]], [r_bank], False)
                if idx > 0:
                    q = k % 3
                    self.mm(bank[:, c0:512], oneg, lbf[q][:, c0:512], False, False, [r_oneg, r_lbf[q]], [r_bank], False)
                zmm(bank, r_bank, hh, qb, kb, False)
                S.op("act", (lambda e: e.activation(out=aT[p][:, c0:512], in_=bank[:, c0:512], func=AF.Exp)), [r_bank], [r_aT[p]])
                if k < 6:
                    self.dump(f"d_a{k}", aT[p], r_aT[p], [128, 512], BF16)
                    if idx > 0:
                        self.dump(f"d_l{k}", lbf[k % 3], r_lbf[k % 3], [128, 512], BF16)

            def stageC(k):
                hh, qb, kb, idx, lastk = blocks[k]
                p = k % 2
                base = hh * 64
                ob = 4 + (qb % 2)
                j = kb - 4 * qb
                c0 = 128 * j if j > 0 else 0
                self.mm(ps[ob][:, c0:512], Vp[sl][:, kb, :], aT[p][:, c0:512], idx == 0, lastk,
                        [r_Vp[sl], r_aT[p]], [ps_r[ob]], True)
                if lastk:
                    S.op("dve", (lambda e: e.tensor_copy(out=ost[sl][base:base + 64, qb * 512:(qb + 1) * 512], in_=ps[ob][base:base + 64, :])),
                         [ps_r[ob]], [r_ost[sl]])
            for step in range(n + 2):
                if step < n: stageA(step)
                if 0 <= step - 1 < n: stageB(step - 1)
                if 0 <= step - 2 < n: stageC(step - 2)
            S.dma("sp", self.YT.ap()[hp * 128:(hp + 1) * 128, b * SEQ:(b + 1) * SEQ], ost[sl], [r_ost[sl]], [self.r["YT"]], f"sbo{sl}")

    def phase_diff(self, l):
        S = self.S; ps = self.ps; ps_r = self.ps_r
        S.new_phase()
        import math, os
        lam_init = 0.8 - 0.6 * math.exp(-0.3 * l)
        idb, r_idb = self.ident_bf, self.r_ident_bf
        onesb, r_onesb = self.ones_bf, self.r_ones_bf
        dl = S.sb([128, 256], F32, "dl"); r_dl = Res()
        S.dma("sp", dl, bass.AP(tensor=self.inp["diff_lambda"], offset=l * 256, ap=[[0, 128], [1, 256]]), [], [r_dl], "dl")
        pr = S.sb([128, 2, 64], F32, "pr"); r_pr = Res()
        S.op("dve", lambda e: e.tensor_tensor(out=pr[:, 0, :], in0=dl[:, 0:64], in1=dl[:, 64:128], op=ALU.mult), [r_dl], [r_pr])
        S.op("dve", lambda e: e.tensor_tensor(out=pr[:, 1, :], in0=dl[:, 128:192], in1=dl[:, 192:256], op=ALU.mult), [r_dl], [r_pr])
        sm = S.sb([128, 4], F32, "sm"); r_sm = Res()
        S.op("dve", lambda e: e.tensor_reduce(out=sm[:, 0:2], in_=pr, axis=AX.X, op=ALU.add), [r_pr], [r_sm])
        S.op("act", lambda e: e.activation(out=sm[:, 2:4], in_=sm[:, 0:2], func=AF.Exp), [r_sm], [r_sm])
        nlam = S.sb([128, 1], F32, "nlam"); r_nlam = Res()
        S.op("dve", lambda e: e.scalar_tensor_tensor(out=nlam, in0=sm[:, 3:4], scalar=-lam_init, in1=sm[:, 2:3], op0=ALU.add, op1=ALU.subtract), [r_sm], [r_nlam])
        gp = S.sb([128, 1], F32, "gp"); r_gp = Res()
        S.dma("sp", gp, self.inp["diff_subln_g"].ap()[l].rearrange("(p o) -> p o", o=1), [], [r_gp], "gp")
        S.op("dve", lambda e: e.tensor_scalar(out=gp, in0=gp, scalar1=(1.0 - lam_init), scalar2=None, op0=ALU.mult), [r_gp], [r_gp])
        QT = [[S.sb([128, SEQ], BF16, "dQT") for _ in range(2)] for _ in range(2)]
        r_QT = [[Res(), Res()], [Res(), Res()]]
        for hh in range(2):
            for sl_ in range(2):
                z0 = (1 - hh) * 64
                S.op("pool", (lambda e, hh=hh, sl_=sl_, z0=z0: e.memset(QT[hh][sl_][z0:z0 + 64, :], 0.0)), [], [r_QT[hh][sl_]])
        KT = [S.sb([128, SEQ], BF16, "dKT") for _ in range(2)]; r_KT = [Res(), Res()]
        Vp = [S.sb([128, 32, 128], BF16, "dVp") for _ in range(2)]; r_Vp = [Res(), Res()]
        yst = S.sb([128, SEQ], BF16, "yst"); r_yst = Res()
        PT = [S.sb([128, 512], BF16, "PT") for _ in range(2)]; r_PT = [Res(), Res()]
        rr = [S.sb([128, 512], F32, "rr") for _ in range(2)]; r_rr = [Res(), Res()]
        oo = S.sb([128, 512], F32, "oo"); r_oo = Res()
        t2 = S.sb([128, 512], F32, "t2"); r_t2 = Res()
        sq = S.sb([128, 512], BF16, "sq"); r_sq = Res()
        rs = S.sb([128, 512], F32, "rs"); r_rs = Res()
        pairs = [(b, dh) for b in range(NSEQ) for dh in range(4)]
        if "DBG" in self.debug:
            pairs = pairs[:int(os.environ.get("DBG_PAIRS", "1"))]

        def load(pi):
            b, dh = pairs[pi]
            sl = pi % 2
            for hh in range(2):
                S.dma("sp", QT[hh][sl][hh * 64:(hh + 1) * 64, :], self.QKT.ap()[1024 + dh * 128 + hh * 64:1024 + dh * 128 + (hh + 1) * 64, b * SEQ:(b + 1) * SEQ],
                      [self.r["QKT"]], [r_QT[hh][sl]], f"dfq{hh}{sl}")
            S.dma("sp", KT[sl], self.QKT.ap()[1536 + dh * 128:1536 + (dh + 1) * 128, b * SEQ:(b + 1) * SEQ], [self.r["QKT"]], [r_KT[sl]], f"dfk{sl}")
            S.dma("sp", Vp[sl], self.V.ap()[b * SEQ:(b + 1) * SEQ, 512 + dh * 128:512 + (dh + 1) * 128].rearrange("(k p) f -> p k f", p=128),
                  [self.r["V"]], [r_Vp[sl]], f"dfv{sl}")
        load(0)
        kglob = 0
        for pi, (b, dh) in enumerate(pairs):
            if pi + 1 < len(pairs):
                load(pi + 1)
            sl = pi % 2
            for qb in range(8):
                for mp in range(2):
                    h = 2 * dh + mp
                    kbs = list(range(4 * qb + 3, -1, -1))
                    n = len(kbs)
                    ob = 2 + mp; db = 4 + mp

                    def stageA(idx, kg):
                        kb = kbs[idx]
                        p = kg % 2
                        j = kb - 4 * qb
                        c0 = 128 * j if j > 0 else 0
                        extra = []
                        if 0 <= j <= 3:
                            extra.append((j, 0))
                        if 0 <= j + 1 <= 3:
                            extra.append((j + 1, 1))
                        self.mm(ps[p][:, c0:512], KT[sl][:, kb * 128:(kb + 1) * 128], QT[mp][sl][:, qb * 512 + c0:(qb + 1) * 512],
                                True, len(extra) == 0, [r_KT[sl], r_QT[mp][sl]], [ps_r[p]], len(extra) == 0)
                        for ei, (qs, d) in enumerate(extra):
                            lastx = ei == len(extra) - 1
                            self.mm(ps[p][:, qs * 128:(qs + 1) * 128], idb, self.Dhi[:, d * 8 + h, :], False, lastx, [r_idb, self.r_Dhi], [ps_r[p]], lastx)
                        S.op("act", (lambda e: e.activation(out=PT[p][:, c0:512], in_=ps[p][:, c0:512], func=AF.Exp, bias=self.b15[:, h:h + 1], scale=1.0)),
                             [ps_r[p], self.r_b15], [r_PT[p]])

                    def stageB(idx, kg):
                        kb = kbs[idx]
                        p = kg % 2
                        j = kb - 4 * qb
                        c0 = 128 * j if j > 0 else 0
                        self.mm(ps[ob][:, c0:512], Vp[sl][:, kb, :], PT[p][:, c0:512], idx == 0, idx == n - 1, [r_Vp[sl], r_PT[p]], [ps_r[ob]], True)
                        self.mm(ps[db][:, c0:512], onesb, PT[p][:, c0:512], idx == 0, idx == n - 1, [r_onesb, r_PT[p]], [ps_r[db]], True)
                    for step in range(n + 1):
                        if step < n: stageA(step, kglob + step)
                        if step >= 1: stageB(step - 1, kglob + step - 1)
                    kglob += n
                    S.op("dve", (lambda e, mp=mp, db=db: e.reciprocal(out=rr[mp], in_=ps[db])), [ps_r[db]], [r_rr[mp]])
                    if mp == 0:
                        S.op("dve", (lambda e, ob=ob: e.tensor_tensor(out=oo, in0=ps[ob], in1=rr[0], op=ALU.mult)), [ps_r[ob], r_rr[0]], [r_oo])
                    else:
                        S.op("dve", (lambda e, ob=ob: e.tensor_tensor(out=t2, in0=ps[ob], in1=rr[1], op=ALU.mult)), [ps_r[ob], r_rr[1]], [r_t2])
                S.op("dve", lambda e: e.scalar_tensor_tensor(out=oo, in0=t2, scalar=nlam[:, 0:1], in1=oo, op0=ALU.mult, op1=ALU.add), [r_t2, r_nlam, r_oo], [r_oo])
                S.op("pool", lambda e: e.tensor_tensor(out=sq, in0=oo, in1=oo, op=ALU.mult), [r_oo], [r_sq])
                self.mm(ps[6], onesb, sq, True, True, [r_onesb, r_sq], [ps_r[6]], True)
                S.op("act", lambda e: e.activation(out=rs, in_=ps[6], func=AF.Sqrt, bias=self.eps_t[:, 0:1], scale=1.0 / 128.0), [ps_r[6], self.r_eps], [r_rs])
                S.op("dve", lambda e: e.reciprocal(out=rs, in_=rs), [r_rs], [r_rs])
                S.op("dve", (lambda e, qb=qb: e.scalar_tensor_tensor(out=yst[:, qb * 512:(qb + 1) * 512], in0=oo, scalar=gp[:, 0:1], in1=rs, op0=ALU.mult, op1=ALU.mult)),
                     [r_oo, r_gp, r_rs], [r_yst])
            S.dma("sp", self.YT.ap()[512 + dh * 128:512 + (dh + 1) * 128, b * SEQ:(b + 1) * SEQ], yst, [r_yst], [self.r["YT"]], "dfo")

    def phase_mem(self, l):
        S = self.S; ps = self.ps; ps_r = self.ps_r
        S.new_phase()
        onesb, r_onesb = self.ones_bf, self.r_ones_bf
        wkv = S.sb([128, 8, D], BF16, "wkv"); r_wkv = Res()
        for c in range(8):
            S.dma("pool", wkv[:, c, :], self.inp["w_mem_kv"].ap()[l, c * 128:(c + 1) * 128, :], [], [r_wkv], "wkv")
        mt = S.sb([128, 2, D], F32, "mt"); r_mt = Res()
        mT = S.sb([128, 8, MEM], BF16, "mT"); r_mT = Res()
        mKT = S.sb([128, 4, MEM], BF16, "mKT"); r_mKT = Res()
        mV = S.sb([128, 2, W], BF16, "mV"); r_mV = Res()
        QT = [S.sb([128, SEQ], BF16, "mQT") for _ in range(2)]; r_QT = [Res(), Res()]
        yst = [S.sb([128, SEQ], BF16, "myst") for _ in range(2)]; r_yst = [Res(), Res()]
        PT = [S.sb([128, 2, 512], BF16, "mPT") for _ in range(2)]; r_PT = [Res(), Res()]
        rr = S.sb([128, 512], F32, "mrr"); r_rr = Res()
        scale = 128.0 ** -0.5
        jobs = [(b, h) for b in range(NSEQ) for h in range(4)]

        def loadq(ji):
            b, h = jobs[ji]
            S.dma("sp", QT[ji % 2], self.QKT.ap()[2048 + h * 128:2048 + (h + 1) * 128, b * SEQ:(b + 1) * SEQ], [self.r["QKT"]], [r_QT[ji % 2]], f"mq{ji % 2}")
        loadq(0)
        ji = 0
        for b in range(NSEQ):
            S.dma("sp", mt, self.inp["mem"].ap()[b * MEM:(b + 1) * MEM, :].rearrange("(s p) d -> p s d", p=128), [], [r_mt], "mt")
            for c in range(8):
                pb = c % 2
                for s in range(2):
                    S.op("pe", (lambda e, pb=pb, s=s, c=c: e.transpose(out=ps[pb][:, s * 128:(s + 1) * 128], in_=mt[:, s, c * 128:(c + 1) * 128], identity=self.ident)),
                         [r_mt, self.r_ident], [ps_r[pb]], inc=(s == 1))
                S.op("dve", (lambda e, pb=pb, c=c: e.tensor_copy(out=mT[:, c, :], in_=ps[pb][:, 0:256])), [ps_r[pb]], [r_mT])
            for h in range(4):
                pb = 2 + h % 2
                for c in range(8):
                    self.mm(ps[pb][:, 0:256], wkv[:, c, h * 128:(h + 1) * 128], mT[:, c, :], c == 0, c == 7, [r_wkv, r_mT], [ps_r[pb]], c == 7)
                S.op("dve", (lambda e, pb=pb, h=h: e.tensor_copy(out=mKT[:, h, :], in_=ps[pb][:, 0:256])), [ps_r[pb]], [r_mKT])
            for mb in range(2):
                pb = 4 + mb
                for c in range(8):
                    self.mm(ps[pb], mT[:, c, mb * 128:(mb + 1) * 128], wkv[:, c, 512:1024], c == 0, c == 7, [r_wkv, r_mT], [ps_r[pb]], c == 7)
                S.op("dve", (lambda e, pb=pb, mb=mb: e.tensor_copy(out=mV[:, mb, :], in_=ps[pb])), [ps_r[pb]], [r_mV])
            for h in range(4):
                if ji + 1 < len(jobs):
                    loadq(ji + 1)
                sl = ji % 2
                for qt in range(8):
                    p = qt % 2
                    for mb in range(2):
                        zb = 0 + mb if p == 0 else 2 + mb
                        self.mm(ps[zb], mKT[:, h, mb * 128:(mb + 1) * 128], QT[sl][:, qt * 512:(qt + 1) * 512], True, True, [r_mKT, r_QT[sl]], [ps_r[zb]], True)
                        S.op("act", (lambda e, zb=zb, p=p, mb=mb: e.activation(out=PT[p][:, mb, :], in_=ps[zb], func=AF.Exp, scale=scale)), [ps_r[zb]], [r_PT[p]])
                    ob = 4 + p; db = 6 + p
                    for mb in range(2):
                        self.mm(ps[ob], mV[:, mb, h * 128:(h + 1) * 128], PT[p][:, mb, :], mb == 0, mb == 1, [r_mV, r_PT[p]], [ps_r[ob]], mb == 1)
                    for mb in range(2):
                        self.mm(ps[db], onesb, PT[p][:, mb, :], mb == 0, mb == 1, [r_onesb, r_PT[p]], [ps_r[db]], mb == 1)
                    S.op("dve", (lambda e, db=db: e.reciprocal(out=rr, in_=ps[db])), [ps_r[db]], [r_rr])
                    S.op("dve", (lambda e, ob=ob, sl=sl, qt=qt: e.tensor_tensor(out=yst[sl][:, qt * 512:(qt + 1) * 512], in0=ps[ob], in1=rr, op=ALU.mult)),
                         [ps_r[ob], r_rr], [r_yst[sl]])
                S.dma("sp", self.YT.ap()[1024 + h * 128:1024 + (h + 1) * 128, b * SEQ:(b + 1) * SEQ], yst[sl], [r_yst[sl]], [self.r["YT"]], f"mo{sl}")
                ji += 1

    def layer_norm(self, zt, r_z, g_t, b_t, r_gb, tmp):
        S = self.S
        st, mv, rstd, r_st = tmp
        for j in range(2):
            S.op("dve", (lambda e, j=j: e.bn_stats(out=st[:, j, :], in_=zt[:, j * 512:(j + 1) * 512])), [r_z], [r_st])
        S.op("dve", lambda e: e.bn_aggr(out=mv, in_=st), [r_st], [r_st])
        S.op("act", lambda e: e.activation(out=rstd, in_=mv[:, 1:2], func=AF.Sqrt, bias=self.eps_t[:, 0:1], scale=1.0), [r_st, self.r_eps], [r_st])
        S.op("dve", lambda e: e.reciprocal(out=rstd, in_=rstd), [r_st], [r_st])
        S.op("dve", lambda e: e.tensor_scalar(out=zt, in0=zt, scalar1=mv[:, 0:1], scalar2=rstd[:, 0:1], op0=ALU.subtract, op1=ALU.mult), [r_z, r_st], [r_z])
        S.op("pool", lambda e: e.tensor_tensor(out=zt, in0=zt, in1=g_t, op=ALU.mult), [r_z, r_gb], [r_z])
        S.op("pool", lambda e: e.tensor_tensor(out=zt, in0=zt, in1=b_t, op=ALU.add), [r_z, r_gb], [r_z])

    def ln_tmp(self):
        S = self.S
        return (S.sb([128, 2, 6], F32, "lnst"), S.sb([128, 2], F32, "lnmv"), S.sb([128, 1], F32, "lnrs"), Res())

    def bcast_row(self, src_tensor, offset, n, name):
        S = self.S
        t = S.sb([128, n], F32, name); r = Res()
        S.dma("sp", t, bass.AP(tensor=src_tensor, offset=offset, ap=[[0, 128], [1, n]]), [], [r], "bc_" + name)
        return t, r

    def phase5(self, l, x_src, r_xsrc):
        S = self.S; ps = self.ps; ps_r = self.ps_r
        S.new_phase()
        wb = S.sb([128, 12, D], BF16, "wb"); r_wb = Res()
        for c in range(0, 12, 4):
            S.dma("pool", wb[:, c:c + 4, :], self.inp["w_branch"].ap()[l, c * 128:(c + 4) * 128, :].rearrange("(c p) n -> p c n", p=128), [], [r_wb], "wb")
        wo = S.sb([128, 8, D], BF16, "wo"); r_wo = Res()
        for c in range(0, 8, 4):
            S.dma("pool", wo[:, c:c + 4, :], self.inp["w_out"].ap()[l, c * 128:(c + 4) * 128, :].rearrange("(c p) n -> p c n", p=128), [], [r_wo], "wo")
        rw = S.sb([128, 8, NE], F32, "rw"); r_rw = Res()
        S.dma("sp", rw, self.inp["router_w"].ap()[l].rearrange("(c p) e -> p c e", p=128), [], [r_rw], "rw")
        rb, r_rb = self.bcast_row(self.inp["router_b"], l * NE, NE, "rb")
        g1, r_g1 = self.bcast_row(self.inp["ln1_g"], l * D, D, "g1")
        b1, r_b1 = self.bcast_row(self.inp["ln1_b"], l * D, D, "b1")
        r_gb = Res()
        S.op("pool", lambda e: e.tensor_copy(out=g1[:, 0:1], in_=g1[:, 0:1]), [r_g1, r_b1], [r_gb])
        yt = [S.sb([128, 12, 512], BF16, "yt") for _ in range(2)]; r_yt = [Res(), Res()]
        gt = S.sb([128, 24, 512], BF16, "gt"); r_gt = Res()
        xt = [S.sb([128, 4, D], F32, "xt5") for _ in range(2)]; r_xt = [Res(), Res()]
        mT = S.sb([128, 8, 512], BF16, "mT"); r_mT = Res()
        mt = [S.sb([128, 512], F32, "mtmp") for _ in range(3)]; r_mt = [Res(), Res(), Res()]
        x1Tb = S.sb([128, 8, 512], BF16, "x1Tb"); r_x1Tb = Res()
        x1Tf = S.sb([128, 8, 512], F32, "x1Tf"); r_x1Tf = Res()
        lg = S.sb([128, NE], F32, "lg"); r_lg = Res()
        t8 = S.sb([128, 16], F32, "t8"); r_t8 = Res()
        ee = S.sb([128, NE], F32, "ee"); r_ee = Res()
        cw = S.sb([128, 4, NE], F32, "cw5"); r_cw = Res()
        cwT = S.sb([NE, 512], F32, "cwT5"); r_cwT = Res()
        lnt = self.ln_tmp()
        NT = T // 512
        if "DBG" in self.debug:
            import os
            NT = int(os.environ.get("DBG_TILES", "2"))

        def load(i):
            t0 = i * 512
            S.dma("sp", yt[i % 2], self.YT.ap()[:, t0:t0 + 512].rearrange("(c p) t -> p c t", p=128), [self.r["YT"]], [r_yt[i % 2]], f"p5y{i % 2}")
            S.dma("sp", xt[i % 2], x_src[t0:t0 + 512, :].rearrange("(s p) d -> p s d", p=128), [r_xsrc], [r_xt[i % 2]], f"p5x{i % 2}")
        load(0)
        pbi = 0
        for i in range(NT):
            t0 = i * 512
            b = i % 2
            S.dma("sp", gt, self.GT.ap()[:, t0:t0 + 512].rearrange("(c p) t -> p c t", p=128), [self.r["GT"]], [r_gt], "p5g")
            if i + 1 < NT:
                load(i + 1)
            for f in range(8):
                banks = []
                for br in range(3):
                    pb = pbi % 8; pbi += 1
                    banks.append(pb)
                    for k in range(4):
                        self.mm(ps[pb], wb[:, br * 4 + k, f * 128:(f + 1) * 128], yt[b][:, br * 4 + k, :], k == 0, k == 3, [r_wb, r_yt[b]], [ps_r[pb]], k == 3)
                for br in range(3):
                    pb = banks[br]
                    S.op("dve", (lambda e, pb=pb, br=br, f=f: e.tensor_tensor(out=mt[br], in0=ps[pb], in1=gt[:, br * 8 + f, :], op=ALU.mult)),
                         [ps_r[pb], r_gt], [r_mt[br]])
                S.op("pool", lambda e: e.tensor_tensor(out=mt[0], in0=mt[0], in1=mt[1], op=ALU.add), [r_mt[0], r_mt[1]], [r_mt[0]])
                S.op("pool", (lambda e, f=f: e.tensor_tensor(out=mT[:, f, :], in0=mt[0], in1=mt[2], op=ALU.add)), [r_mt[0], r_mt[2]], [r_mT])
            if "DBG" in self.debug and i == 0 and l == 0:
                self.dump2("d_mT", mT, r_mT, [128, 8, 512], BF16)
            for s in range(4):
                for hf in range(2):
                    pb = pbi % 8; pbi += 1
                    for f in range(8):
                        self.mm(ps[pb], mT[:, f, s * 128:(s + 1) * 128], wo[:, f, hf * 512:(hf + 1) * 512], f == 0, f == 7, [r_mT, r_wo], [ps_r[pb]], f == 7)
                    S.op("dve", (lambda e, pb=pb, s=s, hf=hf, b=b: e.scalar_tensor_tensor(out=xt[b][:, s, hf * 512:(hf + 1) * 512], in0=xt[b][:, s, hf * 512:(hf + 1) * 512],
                                                                                     scalar=ALPHA, in1=ps[pb], op0=ALU.mult, op1=ALU.add)),
                         [ps_r[pb], r_xt[b]], [r_xt[b]])
                self.layer_norm(xt[b][:, s, :], r_xt[b], g1, b1, r_gb, lnt)
            S.dma("sp", self.X1.ap()[t0:t0 + 512, :].rearrange("(s p) d -> p s d", p=128), xt[b], [r_xt[b]], [self.r["X1"]], f"p5o{b}")
            for c in range(8):
                pb = pbi % 8; pbi += 1
                for s in range(4):
                    S.op("pe", (lambda e, pb=pb, s=s, c=c, b=b: e.transpose(out=ps[pb][:, s * 128:(s + 1) * 128], in_=xt[b][:, s, c * 128:(c + 1) * 128],
                                                                   identity=self.ident)),
                         [r_xt[b], self.r_ident], [ps_r[pb]], inc=(s == 3))
                S.op("dve", (lambda e, pb=pb, c=c: e.tensor_copy(out=x1Tf[:, c, :], in_=ps[pb])), [ps_r[pb]], [r_x1Tf])
                S.op("act", (lambda e, c=c: e.copy(out=x1Tb[:, c, :], in_=x1Tf[:, c, :])), [r_x1Tf], [r_x1Tb])
            S.dma("sp", self.X1T.ap()[:, t0:t0 + 512].rearrange("(c p) t -> p c t", p=128), x1Tb, [r_x1Tb], [self.r["X1T"]], "p5t")
            for s in range(4):
                pb = pbi % 8; pbi += 1
                for c in range(8):
                    self.mm(ps[pb][:, 0:NE], x1Tf[:, c, s * 128:(s + 1) * 128], rw[:, c, :], c == 0, c == 7, [r_x1Tf, r_rw], [ps_r[pb]], c == 7)
                S.op("dve", (lambda e, pb=pb: e.tensor_tensor(out=lg, in0=ps[pb][:, 0:NE], in1=rb, op=ALU.add)), [ps_r[pb], r_rb], [r_lg])
                S.op("dve", lambda e: e.max(out=t8[:, 0:8], in_=lg), [r_lg], [r_t8])
                S.op("dve", lambda e: e.tensor_scalar(out=t8[:, 8:9], in0=t8[:, 0:1], scalar1=-1.0, scalar2=None, op0=ALU.mult), [r_t8], [r_t8])
                S.op("act", lambda e: e.activation(out=ee, in_=lg, func=AF.Exp, bias=t8[:, 8:9], scale=1.0), [r_lg, r_t8], [r_ee])
                S.op("dve", lambda e: e.tensor_scalar(out=lg, in0=lg, scalar1=t8[:, 3:4], scalar2=None, op0=ALU.is_ge), [r_lg, r_t8], [r_lg])
                S.op("dve", lambda e: e.tensor_tensor(out=ee, in0=ee, in1=lg, op=ALU.mult), [r_ee, r_lg], [r_ee])
                S.op("dve", lambda e: e.tensor_reduce(out=t8[:, 9:10], in_=ee, axis=AX.X, op=ALU.add), [r_ee], [r_t8])
                S.op("dve", lambda e: e.reciprocal(out=t8[:, 10:11], in_=t8[:, 9:10]), [r_t8], [r_t8])
                S.op("dve", (lambda e, s=s: e.tensor_scalar(out=cw[:, s, :], in0=ee, scalar1=t8[:, 10:11], scalar2=None, op0=ALU.mult)), [r_ee, r_t8], [r_cw])
                pb2 = pbi % 8; pbi += 1
                self.mm(ps[pb2][0:NE, 0:128], cw[:, s, :], self.ident, True, True, [r_cw, self.r_ident], [ps_r[pb2]], True)
                S.op("dve", (lambda e, pb2=pb2, s=s: e.tensor_copy(out=cwT[:, s * 128:(s + 1) * 128], in_=ps[pb2][0:NE, 0:128])), [ps_r[pb2]], [r_cwT])
            S.dma("sp", self.CW.ap()[t0:t0 + 512, :].rearrange("(s p) e -> p s e", p=128), cw, [r_cw], [self.r["CW"]], "p5c")
            S.dma("sp", self.CWT.ap()[:, t0:t0 + 512], cwT, [r_cwT], [self.r["CWT"]], "p5ct")

    def dump2(self, name, ap, res, shape, dtype):
        t = self.nc.dram_tensor(name, shape, dtype, kind="ExternalOutput")
        self.S.dma("sp", t.ap(), ap, [res], [Res(name, True)], "dbg_" + name)

    def phase_moe(self, l, dst, r_dst):
        S = self.S; ps = self.ps; ps_r = self.ps_r
        S.new_phase()
        import os
        NB = 1024
        NSUB = NB // 128
        NTL = NB // 512
        wgu_d = self.inp["w_gate_up"].ap(); wd_d = self.inp["w_down"].ap()
        braw = S.sb([NE, 2 * D], F32, "braw"); r_braw = Res()
        S.dma("sp", braw, self.inp["b_gate_up"].ap()[l], [], [r_braw], "braw")
        bguT = S.sb([128, 16, NE], F32, "bguT"); r_bguT = Res()
        for j in range(16):
            pb = j % 2
            self.mm(ps[pb][:, 0:NE], braw[:, j * 128:(j + 1) * 128], self.ident[0:NE, 0:NE], True, True, [r_braw, self.r_ident], [ps_r[pb]], True)
            S.op("dve", (lambda e, pb=pb, j=j: e.tensor_copy(out=bguT[:, j, :], in_=ps[pb][:, 0:NE])), [ps_r[pb]], [r_bguT])
        bd = S.sb([NE, D], F32, "bd"); r_bd = Res()
        S.dma("sp", bd, self.inp["b_down"].ap()[l], [], [r_bd], "bd")
        acc = S.sb([128, NSUB, D], F32, "acc"); r_acc = [Res() for _ in range(NSUB)]
        cw = S.sb([128, NSUB, NE], F32, "cw"); r_cw = Res()
        cwT = S.sb([NE, NB], F32, "cwT"); r_cwT = Res()
        x1T = S.sb([128, NTL, 8, 512], BF16, "x1T"); r_x1T = Res()
        wgu = [S.sb([128, 8, 2 * D], BF16, "wgu") for _ in range(2)]; r_wgu = [Res(), Res()]
        wd = [S.sb([128, 8, D], BF16, "wd") for _ in range(2)]; r_wd = [Res(), Res()]
        act = [S.sb([128, 8, 512], BF16, "act") for _ in range(2)]; r_act = [Res(), Res()]
        tmp_off = S.sb_off
        gb = [S.sb([128, 512], F32, "gb") for _ in range(2)]; r_gb = [Res(), Res()]
        sg = [S.sb([128, 512], F32, "sg") for _ in range(2)]; r_sg = [Res(), Res()]
        ub = [S.sb([128, 512], F32, "ub") for _ in range(2)]; r_ub = [Res(), Res()]
        end_off = S.sb_off
        nblocks = T // NB
        nexp = NE
        if "DBG" in self.debug:
            nblocks = int(os.environ.get("DBG_MOEBLK", "1"))

        def loadw(e, nb):
            sl = e % 2
            if nb == 0:
                for c in range(0, 8, 2):
                    S.dma("pool", wgu[sl][:, c:c + 2, :], wgu_d[l, e, c * 128:(c + 2) * 128, :].rearrange("(c p) n -> p c n", p=128), [], [r_wgu[sl]], f"wgu{sl}")
                for c in range(0, 8, 4):
                    S.dma("pool", wd[sl][:, c:c + 4, :], wd_d[l, e, c * 128:(c + 4) * 128, :].rearrange("(c p) n -> p c n", p=128), [], [r_wd[sl]], f"wd{sl}")
                for c in range(0, 8, 4):
                    S.dma("sp", self.WBGU.ap()[e * D + c * 128:e * D + (c + 4) * 128, :].rearrange("(c p) n -> p c n", p=128), wgu[sl][:, c:c + 4, :],
                          [r_wgu[sl]], [self.r["WBGU"]], f"wst{sl}")
                S.dma("sp", self.WBD.ap()[e * D:(e + 1) * D, :].rearrange("(c p) n -> p c n", p=128), wd[sl], [r_wd[sl]], [self.r["WBD"]], f"wst{sl}")
            else:
                for c in range(0, 8, 4):
                    S.dma("sp", wgu[sl][:, c:c + 4, :], self.WBGU.ap()[e * D + c * 128:e * D + (c + 4) * 128, :].rearrange("(c p) n -> p c n", p=128),
                          [self.r["WBGU"]], [r_wgu[sl]], f"wgu{sl}")
                S.dma("sp", wd[sl], self.WBD.ap()[e * D:(e + 1) * D, :].rearrange("(c p) n -> p c n", p=128), [self.r["WBD"]], [r_wd[sl]], f"wd{sl}")
        pbd = 0
        for nb in range(nblocks):
            tb = nb * NB
            loadw(0, nb)
            S.dma("sp", cw, self.CW.ap()[tb:tb + NB, :].rearrange("(s p) e -> p s e", p=128), [self.r["CW"]], [r_cw], "mcw")
            S.dma("sp", cwT, self.CWT.ap()[:, tb:tb + NB], [self.r["CWT"]], [r_cwT], "mcwT")
            for i in range(NTL):
                S.dma("sp", x1T[:, i, :, :], self.X1T.ap()[:, tb + i * 512:tb + (i + 1) * 512].rearrange("(c p) t -> p c t", p=128), [self.r["X1T"]], [r_x1T], "mx1T")
            for s in range(NSUB):
                for hf in range(2):
                    pb = 4 + pbd % 4; pbd += 1
                    self.mm(ps[pb], cwT[:, s * 128:(s + 1) * 128], bd[:, hf * 512:(hf + 1) * 512], True, True, [r_cwT, r_bd], [ps_r[pb]], True)
                    S.op("act", (lambda e, pb=pb, s=s, hf=hf: e.copy(out=acc[:, s, hf * 512:(hf + 1) * 512], in_=ps[pb])), [ps_r[pb]], [r_acc[s]])
            steps = [(e, i) for e in range(nexp) for i in range(NTL)]
            n = len(steps)

            def GU(st):
                e, i = steps[st]
                sl = e % 2; par = st % 2
                for j in range(8):
                    jp = j % 2
                    G = 2 * jp; U = 2 * jp + 1
                    for c in range(8):
                        self.mm(ps[G], wgu[sl][:, c, j * 128:(j + 1) * 128], x1T[:, i, c, :], c == 0, c == 7, [r_wgu[sl], r_x1T], [ps_r[G]], c == 7)
                    for c in range(8):
                        self.mm(ps[U], wgu[sl][:, c, D + j * 128:D + (j + 1) * 128], x1T[:, i, c, :], c == 0, c == 7, [r_wgu[sl], r_x1T], [ps_r[U]], c == 7)
                    S.op("dve", (lambda ee, G=G, jp=jp, j=j, e=e: ee.tensor_scalar(out=gb[jp], in0=ps[G], scalar1=bguT[:, j, e:e + 1], scalar2=7.0, op0=ALU.add, op1=ALU.min)),
                         [ps_r[G], r_bguT], [r_gb[jp]])
                    S.op("act", (lambda ee, jp=jp: ee.activation(out=sg[jp], in_=gb[jp], func=AF.Sigmoid, scale=1.702)), [r_gb[jp]], [r_sg[jp]])
                    S.op("act", (lambda ee, U=U, jp=jp, j=j, e=e: ee.activation(out=ub[jp], in_=ps[U], func=AF.Identity, bias=bguT[:, 8 + j, e:e + 1], scale=1.0)),
                         [ps_r[U], r_bguT], [r_ub[jp]])
                    S.op("dve", (lambda ee, jp=jp: ee.tensor_scalar(out=ub[jp], in0=ub[jp], scalar1=-7.0, scalar2=7.0, op0=ALU.max, op1=ALU.min)), [r_ub[jp]], [r_ub[jp]])
                    S.op("dve", (lambda ee, jp=jp: ee.tensor_tensor(out=gb[jp], in0=gb[jp], in1=sg[jp], op=ALU.mult)), [r_gb[jp], r_sg[jp]], [r_gb[jp]])
                    S.op("dve", (lambda ee, jp=jp, j=j, par=par: ee.scalar_tensor_tensor(out=act[par][:, j, :], in0=ub[jp], scalar=1.0, in1=gb[jp], op0=ALU.add, op1=ALU.mult)),
                         [r_ub[jp], r_gb[jp]], [r_act[par]])

            def DOWN(st):
                nonlocal pbd
                e, i = steps[st]
                sl = e % 2; par = st % 2
                for s in range(4):
                    sub = i * 4 + s
                    for hf in range(2):
                        pb = 4 + pbd % 4; pbd += 1
                        for j in range(8):
                            self.mm(ps[pb], act[par][:, j, s * 128:(s + 1) * 128], wd[sl][:, j, hf * 512:(hf + 1) * 512], j == 0, j == 7, [r_act[par], r_wd[sl]], [ps_r[pb]], j == 7)
                        S.op("dve", (lambda ee, pb=pb, sub=sub, hf=hf, e=e: ee.scalar_tensor_tensor(out=acc[:, sub, hf * 512:(hf + 1) * 512], in0=ps[pb], scalar=cw[:, sub, e:e + 1],
                                                                                           in1=acc[:, sub, hf * 512:(hf + 1) * 512], op0=ALU.mult, op1=ALU.add)),
                             [ps_r[pb], r_cw, r_acc[sub]], [r_acc[sub]])
            for st in range(n + 1):
                if st < n:
                    GU(st)
                    if "DBG" in self.debug and st == 0 and nb == 0 and l == 0:
                        self.dump2("d_act", act[0], r_act[0], [128, 8, 512], BF16)
                        self.dump2("d_gb", gb[1], r_gb[1], [128, 512], F32)
                        self.dump2("d_ub", ub[1], r_ub[1], [128, 512], F32)
                        self.dump2("d_bguT", bguT, r_bguT, [128, 16, NE], F32)
                        self.dump2("d_acc0", acc[:, 0, :], r_acc[0], [128, D], F32)
                if st >= 1:
                    DOWN(st - 1)
                if st < n:
                    e, i = steps[st]
                    if i == 0 and e + 1 < nexp:
                        loadw(e + 1, nb)
            if "DBG" in self.debug and nb == 0 and l == 0:
                self.dump2("d_acc1", acc[:, 0, :], r_acc[0], [128, D], F32)
            S.barrier()
            S.sb_off = tmp_off
            g2, r_g2 = self.bcast_row(self.inp["ln2_g"], l * D, D, "g2")
            b2, r_b2 = self.bcast_row(self.inp["ln2_b"], l * D, D, "b2")
            r_gb2 = Res()
            S.op("pool", lambda e: e.tensor_copy(out=g2[:, 0:1], in_=g2[:, 0:1]), [r_g2, r_b2], [r_gb2])
            lnt = self.ln_tmp()
            assert S.sb_off <= end_off + 4096
            x1t = S.sb([128, 2, D], F32, "x1t"); r_x1t = [Res(), Res()]
            for s in range(NSUB):
                q = s % 2
                S.dma("sp", x1t[:, q, :], self.X1.ap()[tb + s * 128:tb + (s + 1) * 128, :], [self.r["X1"]], [r_x1t[q]], f"mx1{q}")
                S.op("dve", (lambda e, s=s, q=q: e.scalar_tensor_tensor(out=acc[:, s, :], in0=x1t[:, q, :], scalar=ALPHA, in1=acc[:, s, :], op0=ALU.mult, op1=ALU.add)),
                     [r_x1t[q], r_acc[s]], [r_acc[s]])
                self.layer_norm(acc[:, s, :], r_acc[s], g2, b2, r_gb2, lnt)
                S.dma("sp", dst[tb + s * 128:tb + (s + 1) * 128, :], acc[:, s, :], [r_acc[s]], [r_dst], f"mout{s % 4}")
            S.barrier()
            S.sb_off = end_off


def make_in_maps(inputs, with_moe=True):
    consts = host_consts()
    maps = []
    L = DEPTH
    shared = {
        "w_in": np.ascontiguousarray(inputs["w_in"], dtype=np.float32),
        "b_gate": np.ascontiguousarray(inputs["b_gate"], dtype=np.float32),
        "diff_lambda": np.ascontiguousarray(inputs["diff_lambda"], dtype=np.float32).reshape(L, 256),
        "diff_subln_g": np.ascontiguousarray(inputs["diff_subln_g"], dtype=np.float32),
        "rel_bias": np.ascontiguousarray(inputs["rel_bias"], dtype=np.float32),
        "w_mem_kv": np.ascontiguousarray(inputs["w_mem_kv"], dtype=np.float32),
        "w_branch": np.ascontiguousarray(inputs["w_branch"], dtype=np.float32).reshape(L, 3 * W, D),
        "w_out": np.ascontiguousarray(inputs["w_out"], dtype=np.float32),
        "ln1_g": np.ascontiguousarray(inputs["ln1_g"], dtype=np.float32),
        "ln1_b": np.ascontiguousarray(inputs["ln1_b"], dtype=np.float32),
        "router_w": np.ascontiguousarray(inputs["router_w"], dtype=np.float32),
        "router_b": np.ascontiguousarray(inputs["router_b"], dtype=np.float32),
        "w_gate_up": np.ascontiguousarray(inputs["w_gate_up"], dtype=np.float32),
        "b_gate_up": np.ascontiguousarray(inputs["b_gate_up"], dtype=np.float32),
        "w_down": np.ascontiguousarray(inputs["w_down"], dtype=np.float32),
        "b_down": np.ascontiguousarray(inputs["b_down"], dtype=np.float32),
        "ln2_g": np.ascontiguousarray(inputs["ln2_g"], dtype=np.float32),
        "ln2_b": np.ascontiguousarray(inputs["ln2_b"], dtype=np.float32),
    }
    if not with_moe:
        del shared["w_gate_up"], shared["w_down"]
    for k, v in consts.items():
        shared["c_" + k] = v
    x = np.asarray(inputs["x"], dtype=np.float32)
    mem = np.asarray(inputs["mem"], dtype=np.float32)
    for c in range(NCORES):
        m = dict(shared)
        m["x"] = np.ascontiguousarray(x[c * NSEQ:(c + 1) * NSEQ].reshape(T, D))
        m["mem"] = np.ascontiguousarray(mem[c * NSEQ:(c + 1) * NSEQ].reshape(NSEQ * MEM, D))
        maps.append(m)
    return maps


def kernel(**inputs):
    b = Builder()
    maps = make_in_maps(inputs)
    res = run_bass_kernel_spmd(b.nc, maps, core_ids=list(range(NCORES)))
    outs = [np.asarray(r["out"], dtype=np.float32).reshape(NSEQ, SEQ, D) for r in res.results]
    return np.concatenate(outs, axis=0)
```
